# Optimizing a Trainium2 kernel written in Bass

```python
import functools
import jax, jax.numpy as jnp
from jax import lax
import numpy as np

D_MODEL = 1024
BATCH = 1
SEQ = 16384
DEPTH = 1
DEC_BATCH = 128
DEC_SEQ = 4
PAST_LEN = 8192
PAGE_SIZE = 128

N_META = 16
N_HEADS = 8
HEAD_DIM = 64
KV_HEADS = 4
ATTN_DIM = N_HEADS * HEAD_DIM
IDX_HEADS = 8
IDX_DIM = 64
TOPK_MAX = 256
Q_BLOCK = 128
CONV_DIM = D_MODEL // 2
CONV_WIDTH = 3
N_GROUPS = 4
EXPERTS_PER_GROUP = 4
N_EXPERTS = N_GROUPS * EXPERTS_PER_GROUP
TOP_K_IN_GROUP = 2
D_EXPERT = D_MODEL // 4
ROPE_THETA = 10000.0
LN_EPS = 1e-5
DEEPNORM_ALPHA = (2 * DEPTH) ** 0.25
DEEPNORM_BETA = (8 * DEPTH) ** -0.25
IDX_W_SCALE = (IDX_HEADS ** -0.5) * (IDX_DIM ** -0.5)
SPLIT_SIZES = (ATTN_DIM, KV_HEADS * HEAD_DIM, KV_HEADS * HEAD_DIM, IDX_HEADS * IDX_DIM, IDX_HEADS, IDX_DIM,
               CONV_DIM, CONV_DIM, CONV_DIM, D_MODEL, D_MODEL)
PROJ_DIM = sum(SPLIT_SIZES)

kernel_name = 'hybrid_dsa_shortconv_hiermoe_step'


def layer_norm(x, g, b, out_dtype):
    xf = x.astype(jnp.float32)
    mu = jnp.mean(xf, axis=-1, keepdims=True)
    var = jnp.mean(jnp.square(xf - mu), axis=-1, keepdims=True)
    y = (xf - mu) * lax.rsqrt(var + LN_EPS) * g.astype(jnp.float32) + b.astype(jnp.float32)
    return y.astype(out_dtype)


def rope(x, pos):
    d = x.shape[-1]
    half = d // 2
    inv = jnp.power(jnp.float32(ROPE_THETA), -jnp.arange(half, dtype=jnp.float32) * 2.0 / d)
    ang = pos.astype(jnp.float32)[:, None] * inv[None, :]
    cos = jnp.cos(ang)[:, None, :]
    sin = jnp.sin(ang)[:, None, :]
    xf = x.astype(jnp.float32)
    x1, x2 = xf[..., :half], xf[..., half:]
    return jnp.concatenate([x1 * cos - x2 * sin, x1 * sin + x2 * cos], axis=-1).astype(x.dtype)


def take_rows(a, idx):
    return jax.vmap(lambda a_n, i_n: a_n[i_n])(a, idx)


def indexer_scores(iq, iw, ik):
    dots = jnp.einsum('nqhd,nld->nqhl', iq, ik).astype(jnp.float32)
    return jnp.einsum('nqh,nqhl->nql', iw.astype(jnp.float32), jax.nn.relu(dots))


def sparse_attend(q, ksel, vsel, valid):
    n, nq = q.shape[:2]
    qg = q.reshape(n, nq, KV_HEADS, N_HEADS // KV_HEADS, HEAD_DIM)
    s = jnp.einsum('nqhgd,nqkhd->nqhgk', qg, ksel).astype(jnp.float32) * (HEAD_DIM ** -0.5)
    s = jnp.where(valid[:, :, None, None, :], s, -jnp.inf)
    p = jax.nn.softmax(s, axis=-1).astype(vsel.dtype)
    o = jnp.einsum('nqhgk,nqkhd->nqhgd', p, vsel)
    return o.reshape(n, nq, ATTN_DIM)


def prompt_sparse_attention(q, k, v, iq, iw, ik, *, top_k):
    n, t = q.shape[:2]
    nb = -(-t // Q_BLOCK)
    pad = nb * Q_BLOCK - t

    def to_blocks(a):
        a = jnp.pad(a, [(0, 0), (0, pad)] + [(0, 0)] * (a.ndim - 2))
        return jnp.swapaxes(a.reshape((n, nb, Q_BLOCK) + a.shape[2:]), 0, 1)

    kpos = jnp.arange(t, dtype=jnp.int32)
    qpos_b = jnp.arange(nb * Q_BLOCK, dtype=jnp.int32).reshape(nb, Q_BLOCK)

    def block(args):
        qb, iqb, iwb, qpos = args
        sc = indexer_scores(iqb, iwb, ik)
        sc = jnp.where((kpos[None, :] <= qpos[:, None])[None], sc, -jnp.inf)
        _, sel = lax.top_k(sc, top_k)
        valid = sel <= qpos[None, :, None]
        return sparse_attend(qb, take_rows(k, sel), take_rows(v, sel), valid)

    out = lax.map(block, (to_blocks(q), to_blocks(iq), to_blocks(iw), qpos_b))
    return jnp.swapaxes(out, 0, 1).reshape(n, nb * Q_BLOCK, ATTN_DIM)[:, :t]


def sample_sparse_attention(q, k, v, iq, iw, ik, *, cache_k, cache_v, cache_ik, layer, page_table, top_k):
    db, s_new = q.shape[:2]
    page = cache_k.shape[2]
    past_len = page_table.shape[1] * page
    past_ik = cache_ik[layer, page_table].reshape(db, past_len, IDX_DIM)
    ik_all = jnp.concatenate([past_ik, ik.astype(past_ik.dtype)], axis=1)
    qpos = past_len + jnp.arange(s_new, dtype=jnp.int32)
    kpos = jnp.arange(past_len + s_new, dtype=jnp.int32)
    sc = indexer_scores(iq, iw, ik_all)
    sc = jnp.where((kpos[None, :] <= qpos[:, None])[None], sc, -jnp.inf)
    _, sel = lax.top_k(sc, top_k)
    past_idx = jnp.minimum(sel, past_len - 1)
    phys = jnp.take_along_axis(page_table, (past_idx // page).reshape(db, -1), axis=1).reshape(sel.shape)
    off = past_idx % page
    is_new = (sel >= past_len)[..., None, None]
    new_idx = jnp.clip(sel - past_len, 0, s_new - 1)

    def pick(cache, cur):
        old = cache[layer, phys, off]
        return jnp.where(is_new, take_rows(cur, new_idx), old)

    valid = sel <= qpos[None, :, None]
    return sparse_attend(q, pick(cache_k, k), pick(cache_v, v), valid)


def short_conv(cu, cb, cc, w_conv, prev):
    z = cc * cu
    zp = jnp.concatenate([prev.astype(z.dtype), z], axis=1)
    t = z.shape[1]
    y = w_conv[0] * zp[:, 0:t]
    for j in range(1, CONV_WIDTH):
        y = y + w_conv[j] * zp[:, j:j + t]
    return cb * y, zp[:, -(CONV_WIDTH - 1):]


def hier_moe(h, w_group, b_group, w_er, b_er, w_gate, w_up, w_down):
    n, t, d = h.shape
    xf = h.reshape(n * t, d)
    g_logits = (xf @ w_group + b_group).astype(jnp.float32)
    g_sel = jnp.argmax(g_logits, axis=-1)
    g_p = jnp.take_along_axis(jax.nn.softmax(g_logits, axis=-1), g_sel[:, None], axis=1)
    e_logits = (xf @ w_er + b_er).astype(jnp.float32).reshape(-1, N_GROUPS, EXPERTS_PER_GROUP)
    e_logits = jnp.take_along_axis(e_logits, g_sel[:, None, None], axis=1)[:, 0]
    top_p, top_i = lax.top_k(jax.nn.softmax(e_logits, axis=-1), TOP_K_IN_GROUP)
    top_p = top_p / jnp.sum(top_p, axis=-1, keepdims=True) * g_p
    ids = g_sel[:, None] * EXPERTS_PER_GROUP + top_i
    comb = jnp.sum(jax.nn.one_hot(ids, N_EXPERTS, dtype=jnp.float32) * top_p[..., None], axis=1)
    hg = jnp.einsum('md,edf->mef', xf, w_gate)
    hu = jnp.einsum('md,edf->mef', xf, w_up)
    act = jax.nn.silu(hg) * hu * comb[:, :, None].astype(hg.dtype)
    out = jnp.einsum('mef,efd->md', act, w_down)
    return out.reshape(n, t, d).astype(h.dtype)


def trunk_layer(h, pos, conv_prev, attend, lw):
    (w_in, b_in, w_conv, w_attn_up, w_conv_out, w_o, ln1_g, ln1_b,
     w_group, b_group, w_er, b_er, w_gate, w_up, w_down, ln2_g, ln2_b) = lw
    n, t, _ = h.shape
    p = jnp.einsum('ntd,de->nte', h, w_in) + b_in
    offs = np.cumsum(SPLIT_SIZES)[:-1].tolist()
    q, k, v, iq, iw, ik, cu, cb, cc, ga, gb = jnp.split(p, offs, axis=-1)
    q = rope(q.reshape(n, t, N_HEADS, HEAD_DIM), pos)
    k = rope(k.reshape(n, t, KV_HEADS, HEAD_DIM), pos)
    v = v.reshape(n, t, KV_HEADS, HEAD_DIM)
    iq = rope(iq.reshape(n, t, IDX_HEADS, IDX_DIM), pos)
    ik = rope(ik[:, :, None, :], pos)[:, :, 0, :]
    iw = iw * IDX_W_SCALE
    attn_o = attend(q, k, v, iq, iw, ik)
    conv_o, conv_state = short_conv(cu, cb, cc, w_conv, conv_prev)
    a = attn_o @ w_attn_up
    b = conv_o @ w_conv_out
    mix = (jax.nn.sigmoid(ga) * a + jax.nn.sigmoid(gb) * b) @ w_o
    h = layer_norm(DEEPNORM_ALPHA * h + mix, ln1_g, ln1_b, h.dtype)
    h = layer_norm(DEEPNORM_ALPHA * h + hier_moe(h, w_group, b_group, w_er, b_er, w_gate, w_up, w_down),
                   ln2_g, ln2_b, h.dtype)
    return h, k, v, ik, conv_state


def setup_inputs(seed: int = 0) -> dict:
    key = jax.random.key(seed)
    ks = iter(jax.random.split(key, 32))

    def nrm(shape, scale):
        return jax.random.normal(next(ks), shape, jnp.float32) * scale

    n_pages = PAST_LEN // PAGE_SIZE
    n_phys = (DEC_BATCH * n_pages * 5) // 4
    page_table = jax.random.permutation(next(ks), n_phys)[:DEC_BATCH * n_pages]
    page_table = page_table.reshape(DEC_BATCH, n_pages).astype(jnp.int32)
    v_lo = ATTN_DIM + KV_HEADS * HEAD_DIM
    v_hi = v_lo + KV_HEADS * HEAD_DIM
    w_in = nrm((DEPTH, D_MODEL, PROJ_DIM), D_MODEL ** -0.5)
    w_in = w_in.at[:, :, v_lo:v_hi].multiply(DEEPNORM_BETA)
    return {
        'x_prompt': nrm((BATCH, SEQ, D_MODEL), 1.0),
        'x_sample': nrm((DEC_BATCH, DEC_SEQ, D_MODEL), 1.0),
        'cache_k': nrm((DEPTH, n_phys, PAGE_SIZE, KV_HEADS, HEAD_DIM), 1.0),
        'cache_v': nrm((DEPTH, n_phys, PAGE_SIZE, KV_HEADS, HEAD_DIM), 1.0),
        'cache_idx_k': nrm((DEPTH, n_phys, PAGE_SIZE, IDX_DIM), 1.0),
        'state_conv': nrm((DEPTH, DEC_BATCH, CONV_WIDTH - 1, CONV_DIM), 1.0),
        'page_table': page_table,
        'meta_tokens': nrm((N_META, D_MODEL), 1.0),
        'w_in': w_in,
        'b_in': nrm((DEPTH, PROJ_DIM), 0.02),
        'w_conv': nrm((DEPTH, CONV_WIDTH, CONV_DIM), CONV_WIDTH ** -0.5),
        'w_attn_up': nrm((DEPTH, ATTN_DIM, D_MODEL), DEEPNORM_BETA * ATTN_DIM ** -0.5),
        'w_conv_out': nrm((DEPTH, CONV_DIM, D_MODEL), DEEPNORM_BETA * CONV_DIM ** -0.5),
        'w_o': nrm((DEPTH, D_MODEL, D_MODEL), DEEPNORM_BETA * D_MODEL ** -0.5),
        'ln1_g': 1.0 + nrm((DEPTH, D_MODEL), 0.02),
        'ln1_b': nrm((DEPTH, D_MODEL), 0.02),
        'w_group': nrm((DEPTH, D_MODEL, N_GROUPS), D_MODEL ** -0.5),
        'b_group': nrm((DEPTH, N_GROUPS), 0.01),
        'w_expert_router': nrm((DEPTH, D_MODEL, N_EXPERTS), D_MODEL ** -0.5),
        'b_expert_router': nrm((DEPTH, N_EXPERTS), 0.01),
        'w_gate': nrm((DEPTH, N_EXPERTS, D_MODEL, D_EXPERT), D_MODEL ** -0.5),
        'w_up': nrm((DEPTH, N_EXPERTS, D_MODEL, D_EXPERT), DEEPNORM_BETA * D_MODEL ** -0.5),
        'w_down': nrm((DEPTH, N_EXPERTS, D_EXPERT, D_MODEL), DEEPNORM_BETA * D_EXPERT ** -0.5),
        'ln2_g': 1.0 + nrm((DEPTH, D_MODEL), 0.02),
        'ln2_b': nrm((DEPTH, D_MODEL), 0.02),
    }


def reference(x_prompt, x_sample, cache_k, cache_v, cache_idx_k, state_conv, page_table, meta_tokens,
              w_in, b_in, w_conv, w_attn_up, w_conv_out, w_o, ln1_g, ln1_b,
              w_group, b_group, w_expert_router, b_expert_router, w_gate, w_up, w_down, ln2_g, ln2_b):
    bsz, s_p, d = x_prompt.shape
    s_s = x_sample.shape[1]
    past_len = page_table.shape[1] * cache_k.shape[2]
    t_p = s_p + N_META
    h_p = jnp.concatenate([jnp.broadcast_to(meta_tokens.astype(x_prompt.dtype)[None], (bsz, N_META, d)), x_prompt], axis=1)
    h_s = x_sample
    pos_p = jnp.arange(t_p, dtype=jnp.int32)
    pos_s = past_len + jnp.arange(s_s, dtype=jnp.int32)
    topk_p = min(TOPK_MAX, t_p // 4)
    topk_s = min(TOPK_MAX, (past_len + s_s) // 4)
    conv_zero = jnp.zeros((bsz, CONV_WIDTH - 1, CONV_DIM), x_prompt.dtype)
    kp, vp, ikp, cp, ks, vs, iks, cs = [], [], [], [], [], [], [], []
    for l in range(DEPTH):
        lw = (w_in[l], b_in[l], w_conv[l], w_attn_up[l], w_conv_out[l], w_o[l], ln1_g[l], ln1_b[l],
              w_group[l], b_group[l], w_expert_router[l], b_expert_router[l], w_gate[l], w_up[l], w_down[l],
              ln2_g[l], ln2_b[l])
        attend_p = functools.partial(prompt_sparse_attention, top_k=topk_p)
        h_p, k_l, v_l, ik_l, c_l = trunk_layer(h_p, pos_p, conv_zero, attend_p, lw)
        kp.append(k_l); vp.append(v_l); ikp.append(ik_l); cp.append(c_l)
        attend_s = functools.partial(sample_sparse_attention, cache_k=cache_k, cache_v=cache_v,
                                     cache_ik=cache_idx_k, layer=l, page_table=page_table, top_k=topk_s)
        h_s, k_l, v_l, ik_l, c_l = trunk_layer(h_s, pos_s, state_conv[l], attend_s, lw)
        ks.append(k_l); vs.append(v_l); iks.append(ik_l); cs.append(c_l)
    y_prompt = h_p[:, N_META:]
    return (y_prompt, h_s, jnp.stack(kp), jnp.stack(vp), jnp.stack(ikp), jnp.stack(cp),
            jnp.stack(ks), jnp.stack(vs), jnp.stack(iks), jnp.stack(cs))
```

```python
import math
import numpy as np
import concourse.bass as bass
import concourse.mybir as mybir
from concourse.bass_utils import run_bass_kernel_spmd

F32 = mybir.dt.float32
BF16 = mybir.dt.bfloat16
I32 = mybir.dt.int32
ALU = mybir.AluOpType
AF = mybir.ActivationFunctionType
AX = mybir.AxisListType

D = 1024
NCORES = 8
O_Q, O_K, O_V, O_IQ, O_IW, O_IK, O_CU, O_CB, O_CC, O_GA, O_GB, PROJ = (
    0, 512, 768, 1024, 1536, 1544, 1608, 2120, 2632, 3144, 4168, 5192)
LN_EPS = 1e-5
NEG = -1.0e30
MNEG = -30000.0
NIT = 26
ACCUM = True


class Tl:
    def __init__(self, h, name):
        self.h = h
        self.name = name
        self.w = None
        self.r = {}
        self.dsem = None
        self.dcnt = 0

    def __getitem__(self, k):
        return self.h[k]


class View:
    def __init__(self, parent, h):
        self.p = parent
        self.h = h
        self.name = parent.name

    w = property(lambda s: s.p.w, lambda s, v: setattr(s.p, "w", v))
    r = property(lambda s: s.p.r, lambda s, v: setattr(s.p, "r", v))
    dsem = property(lambda s: s.p.dsem, lambda s, v: setattr(s.p, "dsem", v))
    dcnt = property(lambda s: s.p.dcnt, lambda s, v: setattr(s.p, "dcnt", v))

    def __getitem__(self, k):
        return self.h[k]


class Eng:
    def __init__(self, name, e, sem, is_pe=False):
        self.name = name
        self.e = e
        self.sem = sem
        self.cnt = 0
        self.waited = {}
        self.is_pe = is_pe
        self.q = []


class K:
    def __init__(self, nc, cfg):
        self.nc = nc
        self.cfg = cfg
        self.stack = []
        self.dma_sems = []

    def enter(self, cm):
        v = cm.__enter__()
        self.stack.append(cm)
        return v

    def sem(self, name):
        return self.enter(self.nc.semaphore(name))

    def sb(self, name, shape, dt=F32):
        return Tl(self.enter(self.nc.sbuf_tensor("s_" + name, list(shape), dt)), "s_" + name)

    def ps(self, name, shape, dt=F32):
        return Tl(self.enter(self.nc.psum_tensor(name, list(shape), dt)), name)

    def dram(self, name, shape, dt=F32, kind="Internal"):
        return Tl(self.nc.dram_tensor(name, list(shape), dt, kind=kind).ap(), name)

    def _deps(self, eng, reads, writes):
        need = {}
        def add(sv):
            if sv is None:
                return
            s, v = sv
            if need.get(s, (None, 0))[1] < v:
                need[s] = (s, v)
        for t in reads:
            add(t.w)
        for t in writes:
            add(t.w)
            for s, v in t.r.items():
                add((s, v))
        out = []
        for s, v in need.values():
            if eng.is_pe and s is eng.sem:
                continue
            if eng.waited.get(s, 0) >= v:
                continue
            eng.waited[s] = v
            out.append((s, v))
        return out

    def op(self, eng, fn, reads=(), writes=()):
        waits = self._deps(eng, reads, writes)
        eng.cnt += 1
        c = eng.cnt
        sem = eng.sem
        def emit(e):
            for s, v in waits:
                e.wait_ge(s, v)
            fn(e).then_inc(sem, 1)
        eng.q.append(emit)
        for t in reads:
            if t.r.get(sem, 0) < c:
                t.r[sem] = c
        for t in writes:
            t.w = (sem, c)
            t.r = {}

    def dma(self, eng, fn, reads, dst):
        waits = self._deps(eng, reads, [dst])
        if dst.dsem is None:
            dst.dsem = self.sem("d_" + dst.name)
        dst.dcnt += 16
        c = dst.dcnt
        sem = dst.dsem
        def emit(e):
            for s, v in waits:
                e.wait_ge(s, v)
            fn(e).then_inc(sem, 16)
        eng.q.append(emit)
        for t in reads:
            if t.r.get(sem, 0) < c:
                t.r[sem] = c
        dst.w = (sem, c)
        dst.r = {}

    def final_wait(self, eng, tiles):
        waits = self._deps(eng, tiles, [])
        def emit(e):
            for s, v in waits:
                e.wait_ge(s, v)
        eng.q.append(emit)


def build(cfg):
    SEQ = cfg["SEQ"]; NPHYS = cfg["NPHYS"]; NSC = cfg["NSC"]; NPG = cfg["NPG"]
    KP = cfg["KTOP_P"]; KS = cfg["KTOP_S"]
    NQT = SEQ // 128
    NSLOT = NQT // NCORES
    NT = NQT + 1
    TP = SEQ + 16
    LS = NPG * 128 + 4
    NTOKS = NSC * 4
    ALPHA = cfg["ALPHA"]

    nc = bass.Bass("TRN2", target_bir_lowering=False)
    k = K(nc, cfg)

    def din(name, shape, dt=F32):
        return Tl(nc.dram_tensor(name, list(shape), dt, kind="ExternalInput").ap(), name)

    def dout(name, shape, dt=F32):
        return Tl(nc.dram_tensor(name, list(shape), dt, kind="ExternalOutput").ap(), name)

    xall = din("xall", [TP, D])
    cstab_in = din("cstab", [128, NT * 64])
    cstab_s_in = din("cstab_s", [128, 64])
    xhalo = din("xhalo", [NSLOT * 2, D])
    cmask_in = din("cmask", [128, 1024])
    tri_in = din("tri", [128, 128])
    tri4_in = din("tri4", [128, 4])
    ident_in = din("ident", [128, 128])
    pidx_in = din("pidx", [128, 1])
    xs_in = din("xs", [NTOKS, D])
    pt_in = din("pt", [NSC, NPG], I32)
    sconv_in = din("sconv", [NSC * 2, 512])
    ckv_in = din("ckv", [NPHYS * 128, 576])
    w_in = din("w_in", [D, PROJ])
    b_in = din("b_in", [1, PROJ])
    w_conv = din("w_conv", [1, 3 * 512])
    w_a = din("w_a", [512, D])
    w_b = din("w_b", [512, D])
    w_o = din("w_o", [D, D])
    ln_in = din("ln", [4, D])
    w_r = din("w_r", [D, 20])
    b_r = din("b_r", [1, 20])
    w_gate = din("w_gate", [16 * D, 256])
    w_up = din("w_up", [16 * D, 256])
    w_down = din("w_down", [16 * 256, D])
    y_p = dout("y_p", [NSLOT * 128, D])
    k_p = dout("k_p", [NSLOT * 128, 256])
    v_p = dout("v_p", [NSLOT * 128, 256])
    ik_p = dout("ik_p", [NSLOT * 128, 64])
    k_m = dout("k_m", [16, 256])
    v_m = dout("v_m", [16, 256])
    ik_m = dout("ik_m", [16, 64])
    cv_p = dout("cv_p", [2, 512])
    y_s = dout("y_s", [NTOKS, D])
    k_s = dout("k_s", [NTOKS, 256])
    v_s = dout("v_s", [NTOKS, 256])
    ik_s = dout("ik_s", [NTOKS, 64])
    cv_s = dout("cv_s", [NSC * 2, 512])
    outs = [y_p, k_p, v_p, ik_p, k_m, v_m, ik_m, cv_p, y_s, k_s, v_s, ik_s, cv_s]
    KPAD = NT * 128
    kT_scr = k.dram("kT_scr", [64, 4, KPAD], BF16)
    ikT_scr = k.dram("ikT_scr", [64, KPAD], BF16)
    v_scr = k.dram("v_scr", [NT * 128, 260], BF16)
    SPAD = (NPG + 1) * 128
    kTs_scr = k.dram("kTs_scr", [NSC * 64, 4, SPAD], BF16)
    ikTs_scr = k.dram("ikTs_scr", [NSC * 64, SPAD], BF16)
    vs_scr = k.dram("vs_scr", [NSC * SPAD, 260], BF16)
    win_bf = k.dram("win_bf", [D, PROJ], BF16)
    wa_bf = k.dram("wa_bf", [512, D], BF16)
    wb_bf = k.dram("wb_bf", [512, D], BF16)
    wo_bf = k.dram("wo_bf", [D, D], BF16)
    wgu_bf = k.dram("wgu_bf", [16 * D, 512], BF16)
    wd_bf = k.dram("wd_bf", [16 * 256, D], BF16)
    z_scr = k.dram("z_scr", [130, 512], F32)
    zs_scr = k.dram("zs_scr", [NSC * 6, 512], F32)
    at_scr = k.dram("at_scr", [128, 512], F32)
    iw_scr = k.dram("iw_scr", [128, 8], F32)

    PE = Eng("pe", nc.tensor, k.sem("s_pe"), is_pe=True)
    ACT = Eng("act", nc.scalar, k.sem("s_act"))
    DVE = Eng("dve", nc.vector, k.sem("s_dve"))
    SP = Eng("sp", nc.sync, k.sem("s_sp"))
    PL = Eng("pl", nc.gpsimd, k.sem("s_pl"))

    ident = k.sb("ident", [128, 128])
    identb = k.sb("identb", [128, 128], BF16)
    idrep = k.sb("idrep", [128, 512], BF16)
    pidx = k.sb("pidx_sb", [128, 1])
    bsl = [k.sb("bsl0", [128, 512]), k.sb("bsl1", [128, 512])]
    ln_bc = k.sb("ln_bc", [128, 2 * D])
    wconv_bc = k.sb("wconv_bc", [128, 3 * 512])
    br_bc = k.sb("br_bc", [128, 20])
    wr_sb = k.sb("wr_sb", [128, 8, 20])
    Ibuf = k.sb("Ibuf", [128, max(KPAD, PROJ)])
    Bm = k.sb("Bm", [128, max(KPAD, PROJ)], BF16)
    x_tm = k.sb("x_tm", [128, D])
    hT_f = View(x_tm, x_tm.h[:, :].rearrange("p (c q) -> p c q", q=128))
    xT = k.sb("xT", [128, 8, 128], BF16)
    xTh = k.sb("xTh", [128, 8, 2], BF16)
    wbuf = [k.sb("wbuf0", [128, 8, 512], BF16), k.sb("wbuf1", [128, 8, 512], BF16)]
    wdbuf = k.sb("wdbuf", [128, 2, D], BF16)
    pa = k.sb("pa", [128, 512])
    pb = k.sb("pb", [128, 512])
    pc = k.sb("pc", [128, 512])
    pd = k.sb("pd", [128, 512])
    pe_ = k.sb("pe_t", [128, 512])
    cs = k.sb("cs", [128, 2, 32])
    ang = k.sb("ang", [128, 32])
    ang2 = k.sb("ang2", [128, 32])
    qT2 = k.sb("qT2", [64, 8, 128], BF16)
    iqT2 = k.sb("iqT2", [64, 8, 128], BF16)
    iw_sb = k.sb("iw_sb", [128, 8])
    kst = k.sb("kst", [64, 4, 128], BF16)
    ikst = k.sb("ikst", [64, 128], BF16)
    vst = k.sb("vst", [128, 4, 65], BF16)
    ikc = [k.sb("ikc0", [64, 512], BF16), k.sb("ikc1", [64, 512], BF16)]
    rl = [k.sb("rl0", [128, 512]), k.sb("rl1", [128, 512])]
    pT = [k.sb("pT0", [128, 8, 128], BF16), k.sb("pT1", [128, 8, 128], BF16)]
    o_sb = k.sb("o_sb", [128, 8, 65])
    rcp = k.sb("rcp", [128, 8])
    attn_o = k.sb("attn_o", [128, 512])
    sm = k.sb("sm", [128, 64])
    aT = k.sb("aT", [128, 4, 128], BF16)
    cT = k.sb("cT", [128, 4, 128], BF16)
    mT = k.sb("mT", [128, 8, 128], BF16)
    hT_b = k.sb("hT_b", [128, 8, 128], BF16)
    actT = k.sb("actT", [128, 2, 128], BF16)
    h1 = k.sb("h1", [128, D])
    r_tm = k.sb("r_tm", [128, D])
    m_tm = r_tm
    z0 = pd; z1 = pe_; z2 = k.sb("z2", [128, 512])
    lg = k.sb("lg", [128, 20])
    rt = k.sb("rt", [128, 64])
    comb = k.sb("comb", [128, 16])
    pt_i = k.sb("pt_i", [128, NPG], I32)
    pt_f = k.sb("pt_f", [128, NPG])
    rows_i = k.sb("rows_i", [128, NPG], I32)
    NPB = cfg.get("NPB", 2)
    pgset = []
    for i in range(NPB):
        pgset.append((k.sb("pgkv%d" % i, [128, 576]), None, None,
                      k.sb("ksts%d" % i, [64, 4, 128], BF16), k.sb("iksts%d" % i, [64, 128], BF16), k.sb("vsts%d" % i, [128, 4, 65], BF16)))
    rows_l = [rows_i, k.sb("rows_i2", [128, NPG], I32)]
    tri4 = k.sb("tri4_sb", [128, 4])
    oh = k.sb("oh", [128, NSC])
    iwm = k.sb("iwm", [128, NSC, 8])
    sel = k.sb("sel", [128, NSC, 4, 4], BF16)
    B = [k.ps("B%d" % i, [128, 512]) for i in range(8)]

    def load(dst, dst_ap, src, src_ap, eng=SP, **kw):
        k.dma(eng, lambda e: e.dma_start(out=dst_ap, in_=src_ap, **kw), [src], dst)

    def dve(fn, reads, writes):
        k.op(DVE, fn, reads, writes)

    def act(fn, reads, writes):
        k.op(ACT, fn, reads, writes)

    def mm(out_t, out_ap, l_t, l_ap, r_t, r_ap, start, stop, skip=True):
        k.op(PE, lambda e: e.matmul(out_ap, l_ap, r_ap, start=start, stop=stop, skip_group_check=skip),
             [l_t, r_t], [out_t])

    def tr(out_t, out_ap, in_t, in_ap, npart):
        k.op(PE, lambda e: e.transpose(out_ap, in_ap, ident[0:npart, 0:npart]), [in_t, ident], [out_t])

    load(ident, ident[:, :], ident_in, ident_in[:, :])
    load(pidx, pidx[:, :], pidx_in, pidx_in[:, :])
    load(wconv_bc, wconv_bc[:, :], w_conv, w_conv[0:1, :].partition_broadcast(128))
    load(br_bc, br_bc[:, :], b_r, b_r[0:1, :].partition_broadcast(128))
    load(wr_sb, wr_sb[:, :, :], w_r, w_r[:, :].rearrange("(c p) n -> p c n", p=128))
    dve(lambda e: e.tensor_copy(identb[:, :], ident[:, :]), [ident], [identb])
    for r in range(4):
        dve(lambda e, r=r: e.tensor_copy(idrep[:, r * 128:(r + 1) * 128], ident[:, :]), [ident], [idrep])
    dve(lambda e: e.memset(vst[:, :, :], 1.0), [], [vst])
    for st in pgset:
        dve(lambda e, st=st: e.memset(st[5][:, :, :], 1.0), [], [st[5]])

    WST = max(KPAD, PROJ)
    nreg = max(1, min(3, WST // PROJ))
    stg_f = [Tl(Ibuf.h[:, i * PROJ:(i + 1) * PROJ], "stgf%d" % i) for i in range(nreg)]
    stg_b = [Tl(Bm.h[:, i * PROJ:(i + 1) * PROJ], "stgb%d" % i) for i in range(nreg)]
    st_i = [0]

    def conv_w(src, dst, rows, cols, dst_col0=0):
        nb = max(1, PROJ // cols)
        r0 = 0
        while r0 < rows:
            nbk = min(nb, (rows - r0) // 128)
            sf = stg_f[st_i[0] % nreg]; sbb = stg_b[st_i[0] % nreg]
            st_i[0] += 1
            load(sf, sf[:, 0:nbk * cols].rearrange("p (c n) -> p c n", n=cols), src,
                 src[r0:r0 + nbk * 128, 0:cols].rearrange("(c p) n -> p c n", p=128))
            dve(lambda e, sf=sf, sbb=sbb, w=nbk * cols: e.tensor_copy(sbb[:, 0:w], sf[:, 0:w]), [sf], [sbb])
            load(dst, dst[r0:r0 + nbk * 128, dst_col0:dst_col0 + cols].rearrange("(c p) n -> p c n", p=128), sbb,
                 sbb[:, 0:nbk * cols].rearrange("p (c n) -> p c n", n=cols), eng=PL)
            r0 += nbk * 128

    conv_w(w_in, win_bf, D, PROJ)
    conv_w(w_a, wa_bf, 512, D)
    conv_w(w_b, wb_bf, 512, D)
    conv_w(w_o, wo_bf, D, D)
    conv_w(w_gate, wgu_bf, 16 * D, 256, 0)
    conv_w(w_up, wgu_bf, 16 * D, 256, 256)
    conv_w(w_down, wd_bf, 16 * 256, D)
    dve(lambda e: e.memset(Ibuf[:, 0:1], 0.0), [], stg_f + [Ibuf])
    dve(lambda e: e.memset(Bm[:, 0:1], 0.0), [], stg_b + [Bm])
    wkv_v = View(Bm, Bm.h[:, 0:4096].rearrange("p (c n) -> p c n", n=512))
    wik_v = View(Bm, Bm.h[:, 4096:4608].rearrange("p (c n) -> p c n", n=64))
    load(wkv_v, wkv_v[:, :, :], win_bf, win_bf[0:D, O_K:O_K + 512].rearrange("(c p) n -> p c n", p=128))
    load(wik_v, wik_v[:, :, :], win_bf, win_bf[0:D, O_IK:O_IK + 64].rearrange("(c p) n -> p c n", p=128))

    wdbuf2 = View(Ibuf, Ibuf.h[:, 0:1024].bitcast(BF16).rearrange("p (c n) -> p c n", n=1024))
    wb_i = [0]
    bs_i = [0]

    def add_bias(dst_t, dst_ap, src_t, src_ap, nrow, off, n):
        bt = bsl[bs_i[0] % 2]
        bs_i[0] += 1
        load(bt, bt[:, 0:n], b_in, b_in[0:1, off:off + n].partition_broadcast(128))
        dve(lambda e: e.tensor_tensor(dst_ap, src_ap, bt[0:nrow, 0:n], op=ALU.add), [src_t, bt], [dst_t])

    def lin(xT_t, nt, kc, wsrc, row0, col0, ncols, out_t, out_ap, tok0=0, first=True, last=True):
        wbt = wbuf[wb_i[0] % 2]
        wb_i[0] += 1
        load(wbt, wbt[:, 0:kc, 0:ncols], wsrc,
             wsrc[row0:row0 + kc * 128, col0:col0 + ncols].rearrange("(c p) n -> p c n", p=128))
        for c in range(kc):
            mm(out_t, out_ap, xT_t, xT_t[:, c, tok0:tok0 + nt], wbt, wbt[:, c, 0:ncols],
               start=(first and c == 0), stop=(last and c == kc - 1))

    def transpose_tm(src_t, src_cols, nt, ncol_chunks, csz, dst_t, dst_fn, banks=(0, 1)):
        per = 512 // 128
        c = 0
        bi = 0
        while c < ncol_chunks:
            n = min(per, ncol_chunks - c)
            bk = B[banks[bi % len(banks)]]
            bi += 1
            for i in range(n):
                tr(bk, bk[0:csz, i * 128:i * 128 + nt], src_t,
                   src_t[0:nt, src_cols + (c + i) * csz: src_cols + (c + i + 1) * csz], nt)
            for i in range(n):
                dve(lambda e, i=i, c=c, bk=bk: e.tensor_copy(dst_fn(c + i), bk[0:csz, i * 128:i * 128 + nt]),
                    [bk], [dst_t])
            c += n

    def rope_tables(n):
        if n == "s":
            load(cs, cs[:, :, :].rearrange("p a d -> p (a d)"), cstab_s_in, cstab_s_in[:, :])
        else:
            load(cs, cs[:, :, :].rearrange("p a d -> p (a d)"), cstab_in, cstab_in[:, n * 64:(n + 1) * 64])

    def rope(src_t, src_c0, nh, nt, dst_t, dst_c0, t1, t2):
        def v(t, c0, half):
            return t[0:nt, c0:c0 + nh * 64].rearrange("p (h d) -> p h d", d=64)[:, :, half * 32:(half + 1) * 32]
        mc = cs[0:nt, 0:1, :].to_broadcast([nt, nh, 32])
        ms = cs[0:nt, 1:2, :].to_broadcast([nt, nh, 32])
        w1 = t1[0:nt, 0:nh * 32].rearrange("p (h d) -> p h d", d=32)
        w2 = t2[0:nt, 0:nh * 32].rearrange("p (h d) -> p h d", d=32)
        x1 = v(src_t, src_c0, 0); x2 = v(src_t, src_c0, 1)
        o1 = v(dst_t, dst_c0, 0); o2 = v(dst_t, dst_c0, 1)
        dve(lambda e: e.tensor_tensor(w1, x2, ms, op=ALU.mult), [src_t, cs], [t1])
        dve(lambda e: e.tensor_tensor(w2, x1, mc, op=ALU.mult), [src_t, cs], [t2])
        dve(lambda e: e.tensor_tensor(o1, w1, w2, op=ALU.subtract), [t1, t2], [dst_t])
        dve(lambda e: e.tensor_tensor(w1, x1, ms, op=ALU.mult), [src_t, cs], [t1])
        dve(lambda e: e.tensor_tensor(w2, x2, mc, op=ALU.mult), [src_t, cs], [t2])
        dve(lambda e: e.scalar_tensor_tensor(o2, w1, -1.0, w2, op0=ALU.mult, op1=ALU.subtract), [t1, t2], [dst_t])

    def load_xT(src, row0, nt, dst_x, dst_xT):
        load(dst_x, dst_x[0:nt, :], src, src[row0:row0 + nt, :])
        transpose_tm(dst_x, 0, nt, 8, 128, dst_xT, lambda c: dst_xT[:, c, 0:nt], banks=(0, 1))

    def kvik_tile(xT_t, nt, pos_ap, kout, vout, ikout, orow0, kT_dst, ikT_dst, v_dst, kcol0, vrow0, do_out, ws=None):
        ta, tb, tc, tk, tik, tv = ws if ws is not None else (pa, pb, pc, kst, ikst, vst)
        for c in range(8):
            mm(B[2], B[2][0:nt, 0:512], xT_t, xT_t[:, c, 0:nt], wkv_v, wkv_v[:, c, :], c == 0, c == 7)
        for c in range(8):
            mm(B[3], B[3][0:nt, 0:64], xT_t, xT_t[:, c, 0:nt], wik_v, wik_v[:, c, :], c == 0, c == 7)
        add_bias(ta, ta[0:nt, :], B[2], B[2][0:nt, :], nt, O_K, 512)
        add_bias(tb, tb[0:nt, 0:64], B[3], B[3][0:nt, 0:64], nt, O_IK, 64)
        rope_tables(pos_ap)
        rope(ta, 0, 4, nt, tc, 0, pd, pe_)
        rope(tb, 0, 1, nt, tc, 256, pd, pe_)
        if do_out:
            load(kout, kout[orow0:orow0 + nt, :], tc, tc[0:nt, 0:256], eng=PL)
            load(vout, vout[orow0:orow0 + nt, :], ta, ta[0:nt, 256:512], eng=PL)
            load(ikout, ikout[orow0:orow0 + nt, :], tc, tc[0:nt, 256:320], eng=PL)
        transpose_tm(tc, 0, nt, 4, 64, tk, lambda c: tk[:, c, 0:nt], banks=(4, 5))
        transpose_tm(tc, 256, nt, 1, 64, tik, lambda c: tik[:, 0:nt], banks=(4, 5))
        dve(lambda e: e.tensor_copy(tv[0:nt, :, 0:64], ta[0:nt, 256:512].rearrange("p (h d) -> p h d", d=64)), [ta], [tv])
        load(kT_dst[0], kT_dst[1](kcol0, nt), tk, tk[:, :, 0:nt], eng=PL)
        load(ikT_dst[0], ikT_dst[1](kcol0, nt), tik, tik[:, 0:nt], eng=PL)
        load(v_dst[0], v_dst[1](vrow0, nt), tv, tv[0:nt, :, :].rearrange("p h d -> p (h d)"), eng=PL)

    MARK = []
    def mark(name):
        MARK.append((name, PE.cnt, DVE.cnt, ACT.cnt))
    mark('phase0_end')
    pk = (kT_scr, lambda c0, n: kT_scr[:, :, c0:c0 + n])
    pik = (ikT_scr, lambda c0, n: ikT_scr[:, c0:c0 + n])
    pv = (v_scr, lambda r0, n: v_scr[r0:r0 + n, :])
    for n in range(NT):
        nt = 16 if n == 0 else 128
        row0 = 0 if n == 0 else 16 + (n - 1) * 128
        xb, xTb = ((x_tm, xT), (h1, mT))[n % 2]
        wsA = ((pa, pb, pc, kst, ikst, vst), (rl[0], rl[1], attn_o, pgset[0][3], pgset[0][4], pgset[0][5]))[n % 2]
        load_xT(xall, row0, nt, xb, xTb)
        own = (n == 0) or ((n - 1) % 8 == 0)
        if n == 0:
            kvik_tile(xTb, nt, 0, k_m, v_m, ik_m, 0, pk, pik, pv, 0, 0, True, ws=wsA)
        else:
            slot = (n - 1) // 8
            kvik_tile(xTb, nt, n, k_p, v_p, ik_p, slot * 128, pk, pik, pv,
                      16 + (n - 1) * 128, 16 + (n - 1) * 128, own, ws=wsA)

    mark('phaseA_end')
    def indexer(nrow, nkeys, iksrc, iq_t, w_t, w_fn, first):
        ci = 0
        for c0 in range(0, nkeys, 512):
            cn = min(512, nkeys - c0)
            ib = ikc[ci % 2]
            ci += 1
            load(ib, ib[:, 0:cn], iksrc[0], iksrc[1](c0, cn))
            for h in range(8):
                bk = B[h % 2]
                mm(bk, bk[0:nrow, 0:cn], iq_t, iq_t[0:64, h, 0:nrow], ib, ib[:, 0:cn], True, True)
                r = rl[h % 2]
                act(lambda e, r=r, bk=bk, cn=cn: e.activation(r[0:nrow, 0:cn], bk[0:nrow, 0:cn], AF.Relu), [bk], [r])
                w_ap = w_fn(h)
                if first and h == 0:
                    dve(lambda e, r=r, c0=c0, cn=cn, w_ap=w_ap: e.tensor_scalar(Ibuf[0:nrow, c0:c0 + cn], r[0:nrow, 0:cn], w_ap, None, op0=ALU.mult), [r, w_t], [Ibuf])
                else:
                    dve(lambda e, r=r, c0=c0, cn=cn, w_ap=w_ap: e.scalar_tensor_tensor(Ibuf[0:nrow, c0:c0 + cn], r[0:nrow, 0:cn], w_ap, Ibuf[0:nrow, c0:c0 + cn], op0=ALU.mult, op1=ALU.add), [r, w_t, Ibuf], [Ibuf])

    def threshold(nq, nkeys, ktop, mask_fn):
        A_ = sm[0:nq, 0:1]; lo = sm[0:nq, 1:2]; hi = sm[0:nq, 2:3]; mid = sm[0:nq, 3:4]
        cnt = sm[0:nq, 4:5]; prd = sm[0:nq, 5:6]; tmp = sm[0:nq, 6:7]
        dve(lambda e: e.tensor_reduce(A_, Ibuf[0:nq, 0:nkeys], axis=AX.X, op=ALU.max), [Ibuf], [sm])
        dve(lambda e: e.tensor_reduce(tmp, Ibuf[0:nq, 0:nkeys], axis=AX.X, op=ALU.min), [Ibuf], [sm])
        dve(lambda e: e.tensor_scalar(tmp, tmp, -1.0, None, op0=ALU.mult), [sm], [sm])
        dve(lambda e: e.tensor_tensor(A_, A_, tmp, op=ALU.max), [sm], [sm])
        mask_fn(nq, nkeys)
        dve(lambda e: e.tensor_scalar(hi, A_, 1.001, 1e-20, op0=ALU.mult, op1=ALU.add), [sm], [sm])
        dve(lambda e: e.tensor_scalar(lo, hi, -1.0, None, op0=ALU.mult), [sm], [sm])
        for it in range(NIT):
            dve(lambda e: e.tensor_tensor(mid, lo, hi, op=ALU.add), [sm], [sm])
            dve(lambda e: e.tensor_scalar(mid, mid, 0.5, None, op0=ALU.mult), [sm], [sm])
            if ACCUM:
                dve(lambda e: e.memset(cnt, 0.0), [], [sm])
                dve(lambda e: e.tensor_scalar(Bm[0:nq, 0:nkeys], Ibuf[0:nq, 0:nkeys], mid, 0.0, op0=ALU.is_ge, op1=ALU.add, accum_out=cnt), [Ibuf, sm], [Bm, sm])
            else:
                dve(lambda e: e.tensor_scalar(Bm[0:nq, 0:nkeys], Ibuf[0:nq, 0:nkeys], mid, None, op0=ALU.is_ge), [Ibuf, sm], [Bm])
                dve(lambda e: e.reduce_sum(cnt, Bm[0:nq, 0:nkeys], axis=AX.X), [Bm], [sm])
            dve(lambda e: e.tensor_scalar(prd, cnt, float(ktop) - 0.5, None, op0=ALU.is_ge), [sm], [sm])
            dve(lambda e: e.tensor_tensor(tmp, mid, lo, op=ALU.subtract), [sm], [sm])
            dve(lambda e: e.scalar_tensor_tensor(lo, tmp, prd, lo, op0=ALU.mult, op1=ALU.add), [sm], [sm])
            dve(lambda e: e.tensor_tensor(tmp, hi, mid, op=ALU.subtract), [sm], [sm])
            dve(lambda e: e.scalar_tensor_tensor(hi, tmp, prd, mid, op0=ALU.mult, op1=ALU.add), [sm], [sm])
        dve(lambda e: e.tensor_scalar(Bm[0:nq, 0:nkeys], Ibuf[0:nq, 0:nkeys], lo, MNEG, op0=ALU.is_lt, op1=ALU.mult), [Ibuf, sm], [Bm])

    KBLK = 7
    kbv = [View(wbuf[i], wbuf[i].h[0:64, :, :].rearrange("p c n -> p (c n)")[:, 0:4 * KBLK * 128].rearrange("p (g s) -> p g s", g=4))
           for i in range(2)]
    vbv = [View(t, t.h[:, :].bitcast(BF16)[:, 0:KBLK * 260].rearrange("p (t d) -> p t d", d=260)) for t in (h1, r_tm)]

    def attn_core(nq, kcols, ksrc, vsrc, mrow, mr_t, mrhs_ap, q_t, q_fn):
        blocks = []
        for (c0, nk) in kcols:
            if nk == 128 and blocks and blocks[-1][-1][1] == 128 and len(blocks[-1]) < KBLK:
                blocks[-1].append((c0, nk))
            else:
                blocks.append([(c0, nk)])

        def load_block(bi):
            blk = blocks[bi]
            kb = kbv[bi % 2]; vb = vbv[bi % 2]
            c0 = blk[0][0]
            n = sum(x[1] for x in blk)
            load(kb, kb[:, :, 0:n], ksrc[0], ksrc[1](c0, n))
            if blk[0][1] == 128:
                load(vb, vb[:, 0:len(blk), :], vsrc[0], vsrc[1](c0, n).rearrange("(t p) d -> p t d", p=128))
            else:
                load(vb, vb[0:n, 0, :], vsrc[0], vsrc[1](c0, n))

        nkt = len(kcols)
        tiles = []
        for bi, blk in enumerate(blocks):
            for tl, (c0, nk) in enumerate(blk):
                tiles.append((bi, tl, c0, nk, c0 - blk[0][0]))

        def scores(ti):
            bi, tl, c0, nk, off = tiles[ti]
            kb = kbv[bi % 2]
            s0 = B[2 + 2 * (ti % 2)]; s1 = B[3 + 2 * (ti % 2)]
            for sbk in (s0, s1):
                mm(sbk, sbk[0:nk, 0:4 * nq].rearrange("p (r q) -> p r q", q=nq), Bm, Bm[0:mrow, c0:c0 + nk], mr_t, mrhs_ap, True, False)
            for g in range(4):
                sbk = (s0, s1)[g // 2]
                mm(sbk, sbk[0:nk, (g % 2) * 2 * nq:(g % 2) * 2 * nq + 2 * nq].rearrange("p (r q) -> p r q", q=nq), kb, kb[:, g, off:off + nk],
                   q_t, q_fn(g), False, g % 2 == 1)
            p = pT[ti % 2]
            for bj, sbk in enumerate((s0, s1)):
                act(lambda e, bj=bj, sbk=sbk, p=p, nk=nk: e.activation(
                    p[0:nk, 4 * bj:4 * bj + 4, 0:nq], sbk[0:nk, 0:4 * nq].rearrange("p (h q) -> p h q", q=nq), AF.Exp, scale=0.125),
                    [sbk], [p])

        def pv(ti):
            bi, tl, c0, nk, off = tiles[ti]
            vb = vbv[bi % 2]
            p = pT[ti % 2]
            for h in range(8):
                ob = B[6 + h // 4]
                mm(ob, ob[0:nq, (h % 4) * 65:(h % 4) * 65 + 65], p, p[0:nk, h, 0:nq], vb,
                   vb[0:nk, tl, (h // 2) * 65:(h // 2) * 65 + 65], (ti == 0 and h % 4 == 0), ti == nkt - 1)

        load_block(0)
        if len(blocks) > 1:
            load_block(1)
        for ti in range(nkt):
            scores(ti)
            if ti >= 1:
                pv(ti - 1)
                if tiles[ti - 1][0] != tiles[ti][0] and tiles[ti][0] + 1 < len(blocks):
                    load_block(tiles[ti][0] + 1)
        pv(nkt - 1)
        for hb in range(2):
            dve(lambda e, hb=hb: e.tensor_copy(o_sb[0:nq, 4 * hb:4 * hb + 4, :], B[6 + hb][0:nq, 0:260].rearrange("p (h d) -> p h d", d=65)), [B[6 + hb]], [o_sb])
        dve(lambda e: e.reciprocal(rcp[0:nq, :], o_sb[0:nq, :, 64]), [o_sb], [rcp])
        dve(lambda e: e.tensor_tensor(attn_o[0:nq, :].rearrange("p (h d) -> p h d", d=64), o_sb[0:nq, :, 0:64],
                                      rcp[0:nq, :].unsqueeze(2).to_broadcast([nq, 8, 64]), op=ALU.mult), [o_sb, rcp], [attn_o])

    def q_project(xT_t, nt, tok0, pos_ap):
        lin(xT_t, nt, 8, win_bf, 0, O_Q, 512, B[2], B[2][0:nt, 0:512], tok0=tok0)
        lin(xT_t, nt, 8, win_bf, 0, O_IQ, 512, B[3], B[3][0:nt, 0:512], tok0=tok0)
        lin(xT_t, nt, 8, win_bf, 0, O_IW, 8, B[4], B[4][0:nt, 0:8], tok0=tok0)
        add_bias(pa, pa[0:nt, :], B[2], B[2][0:nt, :], nt, O_Q, 512)
        add_bias(pb, pb[0:nt, :], B[3], B[3][0:nt, :], nt, O_IQ, 512)
        add_bias(iw_sb, iw_sb[0:nt, :], B[4], B[4][0:nt, 0:8], nt, O_IW, 8)
        dve(lambda e: e.tensor_scalar(iw_sb[0:nt, :], iw_sb[0:nt, :], cfg["IDX_W_SCALE"], None, op0=ALU.mult), [iw_sb], [iw_sb])
        rope_tables(pos_ap)
        rope(pa, 0, 8, nt, pc, 0, pd, pe_)
        transpose_tm(pc, 0, nt, 8, 64, qT2, lambda c: qT2[:, c, 0:nt], banks=(4, 5))
        rope(pb, 0, 8, nt, pc, 0, pd, pe_)
        transpose_tm(pc, 0, nt, 8, 64, iqT2, lambda c: iqT2[:, c, 0:nt], banks=(4, 5))

    def layer_norm(src, nt, which, dst):
        for a in range(2):
            load(ln_bc, ln_bc[:, a * D:(a + 1) * D], ln_in, ln_in[2 * which + a:2 * which + a + 1, :].partition_broadcast(128))
        g_ap = ln_bc[0:nt, 0:D]
        b_ap = ln_bc[0:nt, D:2 * D]
        s = sm[0:nt, 8:9]; ss = sm[0:nt, 9:10]
        dve(lambda e: e.reduce_sum(s, src[0:nt, :], axis=AX.X), [src], [sm])
        dve(lambda e: e.tensor_scalar(s, s, -1.0 / D, None, op0=ALU.mult), [sm], [sm])
        dve(lambda e: e.tensor_scalar(src[0:nt, :], src[0:nt, :], s, None, op0=ALU.add), [src, sm], [src])
        act(lambda e: e.activation(dst[0:nt, :], src[0:nt, :], AF.Square, accum_out=ss), [src], [dst, sm])
        dve(lambda e: e.tensor_scalar(ss, ss, 1.0 / D, LN_EPS, op0=ALU.mult, op1=ALU.add), [sm], [sm])
        act(lambda e: e.activation(ss, ss, AF.Sqrt), [sm], [sm])
        dve(lambda e: e.reciprocal(ss, ss), [sm], [sm])
        dve(lambda e: e.tensor_scalar(src[0:nt, :], src[0:nt, :], ss, None, op0=ALU.mult), [src, sm], [src])
        dve(lambda e: e.tensor_tensor(src[0:nt, :], src[0:nt, :], g_ap, op=ALU.mult), [src, ln_bc], [src])
        dve(lambda e: e.tensor_tensor(dst[0:nt, :], src[0:nt, :], b_ap, op=ALU.add), [src, ln_bc], [dst])

    def tail(nt, xT_t, tok0, zsrc_fn, y_out, yrow0):
        lin(xT_t, nt, 8, win_bf, 0, O_CU, 512, B[2], B[2][0:nt, :], tok0=tok0)
        lin(xT_t, nt, 8, win_bf, 0, O_CC, 512, B[3], B[3][0:nt, :], tok0=tok0)
        lin(xT_t, nt, 8, win_bf, 0, O_CB, 512, B[4], B[4][0:nt, :], tok0=tok0)
        add_bias(pa, pa[0:nt, :], B[2], B[2][0:nt, :], nt, O_CU, 512)
        add_bias(pb, pb[0:nt, :], B[3], B[3][0:nt, :], nt, O_CC, 512)
        dve(lambda e: e.tensor_tensor(z2[0:nt, :], pa[0:nt, :], pb[0:nt, :], op=ALU.mult), [pa, pb], [z2])
        zsrc_fn(nt)
        dve(lambda e: e.tensor_tensor(pa[0:nt, :], z0[0:nt, :], wconv_bc[0:nt, 0:512], op=ALU.mult), [z0, wconv_bc], [pa])
        dve(lambda e: e.tensor_tensor(pb[0:nt, :], z1[0:nt, :], wconv_bc[0:nt, 512:1024], op=ALU.mult), [z1, wconv_bc], [pb])
        dve(lambda e: e.tensor_tensor(pa[0:nt, :], pa[0:nt, :], pb[0:nt, :], op=ALU.add), [pa, pb], [pa])
        dve(lambda e: e.tensor_tensor(pb[0:nt, :], z2[0:nt, :], wconv_bc[0:nt, 1024:1536], op=ALU.mult), [z2, wconv_bc], [pb])
        dve(lambda e: e.tensor_tensor(pa[0:nt, :], pa[0:nt, :], pb[0:nt, :], op=ALU.add), [pa, pb], [pa])
        add_bias(pb, pb[0:nt, :], B[4], B[4][0:nt, :], nt, O_CB, 512)
        dve(lambda e: e.tensor_tensor(pc[0:nt, :], pa[0:nt, :], pb[0:nt, :], op=ALU.mult), [pa, pb], [pc])
        transpose_tm(pc, 0, nt, 4, 128, cT, lambda c: cT[:, c, 0:nt], banks=(0, 1))
        transpose_tm(attn_o, 0, nt, 4, 128, aT, lambda c: aT[:, c, 0:nt], banks=(0, 1))
        for half in range(2):
            c0 = half * 512
            lin(aT, nt, 4, wa_bf, 0, c0, 512, B[2], B[2][0:nt, :])
            lin(cT, nt, 4, wb_bf, 0, c0, 512, B[3], B[3][0:nt, :])
            lin(xT_t, nt, 8, win_bf, 0, O_GA + c0, 512, B[4], B[4][0:nt, :], tok0=tok0)
            lin(xT_t, nt, 8, win_bf, 0, O_GB + c0, 512, B[5], B[5][0:nt, :], tok0=tok0)
            add_bias(pa, pa[0:nt, :], B[4], B[4][0:nt, :], nt, O_GA + c0, 512)
            add_bias(pb, pb[0:nt, :], B[5], B[5][0:nt, :], nt, O_GB + c0, 512)
            act(lambda e: e.activation(pa[0:nt, :], pa[0:nt, :], AF.Sigmoid), [pa], [pa])
            act(lambda e: e.activation(pb[0:nt, :], pb[0:nt, :], AF.Sigmoid), [pb], [pb])
            dve(lambda e: e.tensor_tensor(pa[0:nt, :], pa[0:nt, :], B[2][0:nt, :], op=ALU.mult), [pa, B[2]], [pa])
            dve(lambda e: e.tensor_tensor(pb[0:nt, :], pb[0:nt, :], B[3][0:nt, :], op=ALU.mult), [pb, B[3]], [pb])
            dve(lambda e, c0=c0: e.tensor_tensor(m_tm[0:nt, c0:c0 + 512], pa[0:nt, :], pb[0:nt, :], op=ALU.add), [pa, pb], [m_tm])
        transpose_tm(m_tm, 0, nt, 8, 128, mT, lambda c: mT[:, c, 0:nt], banks=(0, 1))
        for half in range(2):
            c0 = half * 512
            lin(mT, nt, 8, wo_bf, 0, c0, 512, B[2 + half], B[2 + half][0:nt, :])
            dve(lambda e, c0=c0, half=half: e.scalar_tensor_tensor(r_tm[0:nt, c0:c0 + 512], x_tm[0:nt, c0:c0 + 512], ALPHA, B[2 + half][0:nt, :], op0=ALU.mult, op1=ALU.add), [x_tm, B[2 + half]], [r_tm])
        layer_norm(r_tm, nt, 0, h1)
        transpose_tm(h1, 0, nt, 8, 128, hT_f, lambda c: hT_f[:, c, 0:nt], banks=(0, 1))
        dve(lambda e: e.tensor_copy(hT_b[:, :, 0:nt], hT_f[:, :, 0:nt]), [hT_f], [hT_b])
        for c in range(8):
            mm(B[2], B[2][0:nt, 0:20], hT_f, hT_f[:, c, 0:nt], wr_sb, wr_sb[:, c, :], c == 0, c == 7)
        dve(lambda e: e.tensor_tensor(lg[0:nt, :], B[2][0:nt, 0:20], br_bc[0:nt, :], op=ALU.add), [B[2], br_bc], [lg])
        R = lambda a, b: rt[0:nt, a:b]
        gl = lg[0:nt, 0:4]
        dve(lambda e: e.tensor_reduce(R(0, 1), gl, axis=AX.X, op=ALU.max), [lg], [rt])
        dve(lambda e: e.tensor_scalar(R(1, 5), gl, R(0, 1), None, op0=ALU.is_ge), [lg, rt], [rt])
        dve(lambda e: e.tensor_scalar(R(5, 9), gl, R(0, 1), None, op0=ALU.subtract), [lg, rt], [rt])
        act(lambda e: e.activation(R(5, 9), R(5, 9), AF.Exp, accum_out=R(9, 10)), [rt], [rt])
        dve(lambda e: e.reciprocal(R(10, 11), R(9, 10)), [rt], [rt])
        dve(lambda e: e.tensor_tensor(R(16, 32).rearrange("p (g j) -> p g j", j=4), lg[0:nt, 4:20].rearrange("p (g j) -> p g j", j=4),
                                      R(1, 5).unsqueeze(2).to_broadcast([nt, 4, 4]), op=ALU.mult), [lg, rt], [rt])
        dve(lambda e: e.tensor_reduce(R(32, 36), R(16, 32).rearrange("p (g j) -> p j g", j=4), axis=AX.X, op=ALU.add), [rt], [rt])
        dve(lambda e: e.tensor_reduce(R(36, 37), R(32, 36), axis=AX.X, op=ALU.max), [rt], [rt])
        dve(lambda e: e.tensor_scalar(R(37, 41), R(32, 36), R(36, 37), NEG, op0=ALU.is_ge, op1=ALU.mult), [rt], [rt])
        dve(lambda e: e.tensor_tensor(R(37, 41), R(37, 41), R(32, 36), op=ALU.add), [rt], [rt])
        dve(lambda e: e.tensor_reduce(R(41, 42), R(37, 41), axis=AX.X, op=ALU.max), [rt], [rt])
        dve(lambda e: e.tensor_scalar(R(42, 46), R(32, 36), R(41, 42), None, op0=ALU.is_ge), [rt], [rt])
        dve(lambda e: e.tensor_scalar(R(46, 50), R(32, 36), R(36, 37), None, op0=ALU.subtract), [rt], [rt])
        act(lambda e: e.activation(R(46, 50), R(46, 50), AF.Exp), [rt], [rt])
        dve(lambda e: e.tensor_tensor(R(46, 50), R(46, 50), R(42, 46), op=ALU.mult), [rt], [rt])
        dve(lambda e: e.reduce_sum(R(50, 51), R(46, 50), axis=AX.X), [rt], [rt])
        dve(lambda e: e.reciprocal(R(51, 52), R(50, 51)), [rt], [rt])
        dve(lambda e: e.tensor_tensor(R(51, 52), R(51, 52), R(10, 11), op=ALU.mult), [rt], [rt])
        dve(lambda e: e.tensor_scalar(R(46, 50), R(46, 50), R(51, 52), None, op0=ALU.mult), [rt], [rt])
        dve(lambda e: e.tensor_tensor(comb[0:nt, :].rearrange("p (g j) -> p g j", j=4), R(1, 5).unsqueeze(2).to_broadcast([nt, 4, 4]),
                                      R(46, 50).unsqueeze(1).to_broadcast([nt, 4, 4]), op=ALU.mult), [rt], [comb])
        for ex in range(16):
            ea, eb, eT, ewd = ((pa, pb, actT, wdbuf), (rl[0], rl[1], aT, wdbuf2))[ex % 2]
            lin(hT_b, nt, 8, wgu_bf, ex * D, 0, 512, B[2 + ex % 2], B[2 + ex % 2][0:nt, :])
            gu = B[2 + ex % 2]
            load(ewd, ewd[:, :, :], wd_bf, wd_bf[ex * 256:(ex + 1) * 256, :].rearrange("(c p) n -> p c n", p=128))
            act(lambda e, gu=gu, ea=ea: e.activation(ea[0:nt, 0:256], gu[0:nt, 0:256], AF.Silu), [gu], [ea])
            dve(lambda e, gu=gu, ex=ex, ea=ea, eb=eb: e.scalar_tensor_tensor(eb[0:nt, 0:256], gu[0:nt, 256:512], comb[0:nt, ex:ex + 1], ea[0:nt, 0:256], op0=ALU.mult, op1=ALU.mult), [gu, comb, ea], [eb])
            transpose_tm(eb, 0, nt, 2, 128, eT, lambda c, eT=eT: eT[:, c, 0:nt], banks=((4,), (5,))[ex % 2])
            for half in range(2):
                for f in range(2):
                    mm(B[6 + half], B[6 + half][0:nt, :], eT, eT[:, f, 0:nt], ewd, ewd[:, f, half * 512:(half + 1) * 512],
                       ex == 0 and f == 0, ex == 15 and f == 1)
        for half in range(2):
            c0 = half * 512
            dve(lambda e, c0=c0, half=half: e.scalar_tensor_tensor(r_tm[0:nt, c0:c0 + 512], h1[0:nt, c0:c0 + 512], ALPHA, B[6 + half][0:nt, :], op0=ALU.mult, op1=ALU.add), [h1, B[6 + half]], [r_tm])
        layer_norm(r_tm, nt, 1, h1)
        load(y_out, y_out[yrow0:yrow0 + nt, :], h1, h1[0:nt, :], eng=PL)

    def prompt_mask(nq, nkeys):
        for hh in range(2):
            t = (pa, pb)[hh]
            c0 = nkeys - 1024 + hh * 512
            load(t, t[:, :], cmask_in, cmask_in[:, hh * 512:(hh + 1) * 512])
            dve(lambda e, t=t, c0=c0: e.tensor_tensor(Ibuf[0:nq, c0:c0 + 512], Ibuf[0:nq, c0:c0 + 512], t[0:nq, :], op=ALU.add), [Ibuf, t], [Ibuf])

    for j in range(NSLOT):
        n = 1 + 8 * j
        load_xT(xall, 16 + (n - 1) * 128, 128, x_tm, xT)
        mark('slot%d_start' % j)
        q_project(xT, 128, 0, n)
        mark('slot%d_qproj' % j)
        kcols = [(0, 16)] + [(16 + 128 * (t - 1), 128) for t in range(1, 8 * (j + 1) + 1)]
        nkeys = kcols[-1][0] + kcols[-1][1]
        indexer(128, nkeys, pik, iqT2, iw_sb, lambda h: iw_sb[0:128, h:h + 1], True)
        mark('slot%d_indexer' % j)
        threshold(128, nkeys, KP, prompt_mask)
        mark('slot%d_thr' % j)
        attn_core(128, kcols, pk, pv, 128, idrep, idrep[:, :].rearrange("p (r q) -> p r q", q=128), qT2,
                  lambda g: qT2[:, 2 * g:2 * g + 2, 0:128])

        def zsrc(nt, j=j):
            load(m_tm, m_tm[0:2, :], xhalo, xhalo[2 * j:2 * j + 2, :])
            transpose_tm(m_tm, 0, 2, 8, 128, xTh, lambda c: xTh[:, c, 0:2], banks=(0, 1))
            lin(xTh, 2, 8, win_bf, 0, O_CU, 512, B[0], B[0][0:2, :])
            lin(xTh, 2, 8, win_bf, 0, O_CC, 512, B[1], B[1][0:2, :])
            add_bias(pa, pa[0:2, :], B[0], B[0][0:2, :], 2, O_CU, 512)
            add_bias(pb, pb[0:2, :], B[1], B[1][0:2, :], 2, O_CC, 512)
            dve(lambda e: e.tensor_tensor(pa[0:2, :], pa[0:2, :], pb[0:2, :], op=ALU.mult), [pa, pb], [pa])
            load(z_scr, z_scr[0:2, :], pa, pa[0:2, :], eng=PL)
            load(z_scr, z_scr[2:130, :], z2, z2[0:128, :], eng=PL)
            load(z0, z0[0:128, :], z_scr, z_scr[0:128, :])
            load(z1, z1[0:128, :], z_scr, z_scr[1:129, :])
            if j == NSLOT - 1:
                load(cv_p, cv_p[:, :], z_scr, z_scr[128:130, :], eng=PL)
        mark('slot%d_attn' % j)
        tail(128, xT, 0, zsrc, y_p, j * 128)
        mark('slot%d_tail' % j)

    load(x_tm, x_tm[0:NTOKS, :], xs_in, xs_in[:, :])
    transpose_tm(x_tm, 0, NTOKS, 8, 128, xT, lambda c: xT[:, c, 0:NTOKS], banks=(0, 1))
    lin(xT, NTOKS, 8, win_bf, 0, O_K, 512, B[2], B[2][0:NTOKS, 0:512])
    lin(xT, NTOKS, 8, win_bf, 0, O_IK, 64, B[3], B[3][0:NTOKS, 0:64])
    add_bias(pa, pa[0:NTOKS, :], B[2], B[2][0:NTOKS, :], NTOKS, O_K, 512)
    add_bias(pb, pb[0:NTOKS, 0:64], B[3], B[3][0:NTOKS, 0:64], NTOKS, O_IK, 64)
    rope_tables("s")
    rope(pa, 0, 4, NTOKS, pc, 0, pd, pe_)
    rope(pb, 0, 1, NTOKS, pc, 256, pd, pe_)
    load(k_s, k_s[:, :], pc, pc[0:NTOKS, 0:256], eng=PL)
    load(v_s, v_s[:, :], pa, pa[0:NTOKS, 256:512], eng=PL)
    load(ik_s, ik_s[:, :], pc, pc[0:NTOKS, 256:320], eng=PL)
    transpose_tm(pc, 0, NTOKS, 4, 64, kst, lambda c: kst[:, c, 0:NTOKS], banks=(4, 5))
    transpose_tm(pc, 256, NTOKS, 1, 64, ikst, lambda c: ikst[:, 0:NTOKS], banks=(4, 5))
    dve(lambda e: e.tensor_copy(vst[0:NTOKS, :, 0:64], pa[0:NTOKS, 256:512].rearrange("p (h d) -> p h d", d=64)), [pa], [vst])
    for b in range(NSC):
        load(kTs_scr, kTs_scr[b * 64:(b + 1) * 64, :, NPG * 128:NPG * 128 + 4], kst, kst[:, :, 4 * b:4 * b + 4], eng=PL)
        load(ikTs_scr, ikTs_scr[b * 64:(b + 1) * 64, NPG * 128:NPG * 128 + 4], ikst, ikst[:, 4 * b:4 * b + 4], eng=PL)
        load(vs_scr, vs_scr[b * SPAD + NPG * 128:b * SPAD + NPG * 128 + 4, :], vst, vst[4 * b:4 * b + 4, :, :].rearrange("p h d -> p (h d)"), eng=PL)
    q_project(xT, NTOKS, 0, "s")
    qTs = mT
    iqTs = hT_b
    dve(lambda e: e.tensor_copy(qTs[0:64, :, 0:NTOKS], qT2[:, :, 0:NTOKS]), [qT2], [qTs])
    dve(lambda e: e.tensor_copy(iqTs[0:64, :, 0:NTOKS], iqT2[:, :, 0:NTOKS]), [iqT2], [iqTs])

    load(tri4, tri4[:, :], tri4_in, tri4_in[:, :])
    dve(lambda e: e.tensor_reduce(oh[0:NTOKS, :], ident[0:NTOKS, 0:NTOKS].rearrange("p (b q) -> p b q", q=4), axis=AX.X, op=ALU.add), [ident], [oh])
    dve(lambda e: e.tensor_tensor(iwm[0:NTOKS, :, :], iw_sb[0:NTOKS, :].unsqueeze(1).to_broadcast([NTOKS, NSC, 8]),
                                  oh[0:NTOKS, :].unsqueeze(2).to_broadcast([NTOKS, NSC, 8]), op=ALU.mult), [iw_sb, oh], [iwm])
    for r in range(4):
        dve(lambda e, r=r: e.tensor_copy(sel[0:NTOKS, :, r, :], ident[0:NTOKS, 0:NTOKS].rearrange("p (b q) -> p b q", q=4)), [ident], [sel])

    def sample_mask(nq, nkeys):
        dve(lambda e: e.tensor_tensor(Ibuf[0:nq, nkeys - 4:nkeys], Ibuf[0:nq, nkeys - 4:nkeys], tri4[0:nq, 0:4], op=ALU.add), [Ibuf, tri4], [Ibuf])

    SETW = 2064
    nextra = min(6, max(KPAD, PROJ) // SETW)
    xsets = []
    for i in range(nextra):
        o = i * SETW
        xsets.append((Tl(Bm.h[:, o:o + 1152].bitcast(F32), "xg%d" % i), None, None,
                      Tl(Bm.h[0:64, o + 1152:o + 1664].rearrange("p (g s) -> p g s", g=4), "xk%d" % i),
                      Tl(Bm.h[0:64, o + 1664:o + 1792], "xi%d" % i),
                      Tl(Bm.h[:, o + 1792:o + 2052].rearrange("p (h d) -> p h d", d=65), "xv%d" % i)))
    xflat = [t for st in xsets for t in st if t is not None]
    dve(lambda e: e.memset(Bm[:, 0:1], 0.0), [], [Bm] + xflat)
    for st in xsets:
        dve(lambda e, st=st: e.memset(st[5][:, :, :], 1.0), [], [st[5]])
    allsets = pgset + xsets
    NPB_ALL = len(allsets)
    pgc = 0
    for b in range(NSC):
        load(pt_i, pt_i[:, :], pt_in, pt_in[b:b + 1, :].partition_broadcast(128))
        dve(lambda e: e.tensor_copy(pt_f[:, :], pt_i[:, :]), [pt_i], [pt_f])
        dve(lambda e: e.tensor_scalar(pt_f[:, :], pt_f[:, :], 128.0, pidx[:, 0:1], op0=ALU.mult, op1=ALU.add), [pt_f, pidx], [pt_f])
        rws = rows_l[b % 2]
        dve(lambda e, rws=rws: e.tensor_copy(rws[:, :], pt_f[:, :]), [pt_f], [rws])
        for pg in range(NPG):
            st = allsets[pgc % NPB_ALL]
            pgc += 1
            gk, gv, gi, ks_, iks_, vs_ = st
            k.dma(PL, lambda e, dst=gk, pg=pg, rws=rws: e.indirect_dma_start(
                out=dst[:, :], out_offset=None, in_=ckv_in[:, :],
                in_offset=bass.IndirectOffsetOnAxis(ap=rws[:, pg:pg + 1], axis=0)), [ckv_in, rws], gk)
            bk2 = (4, 5) if pg % 2 == 0 else (2, 3)
            transpose_tm(gk, 0, 128, 4, 64, ks_, lambda c, ks_=ks_: ks_[:, c, 0:128], banks=(bk2[0],))
            transpose_tm(gk, 512, 128, 1, 64, iks_, lambda c, iks_=iks_: iks_[:, 0:128], banks=(bk2[1],))
            dve(lambda e, vs_=vs_, gk=gk: e.tensor_copy(vs_[:, :, 0:64], gk[:, 256:512].rearrange("p (h d) -> p h d", d=64)), [gk], [vs_])
            load(kTs_scr, kTs_scr[b * 64:(b + 1) * 64, :, pg * 128:(pg + 1) * 128], ks_, ks_[:, :, :])
            load(ikTs_scr, ikTs_scr[b * 64:(b + 1) * 64, pg * 128:(pg + 1) * 128], iks_, iks_[:, :])
            load(vs_scr, vs_scr[b * SPAD + pg * 128:b * SPAD + (pg + 1) * 128, :], vs_, vs_[:, :, :].rearrange("p h d -> p (h d)"))
        siks = (ikTs_scr, lambda c0, n, b=b: ikTs_scr[b * 64:(b + 1) * 64, c0:c0 + n])
        indexer(NTOKS, LS, siks, iqTs, iwm, lambda h, b=b: iwm[0:NTOKS, b, h:h + 1], b == 0)
    dve(lambda e: e.memset(Bm[:, 0:1], 0.0), [], [Bm] + xflat)
    mark('s_pages_idx')
    threshold(NTOKS, LS, KS, sample_mask)
    mark('s_thr')
    for b in range(NSC):
        kcols = [(128 * t, 128) for t in range(NPG)] + [(NPG * 128, 4)]
        sks = (kTs_scr, lambda c0, n, b=b: kTs_scr[b * 64:(b + 1) * 64, :, c0:c0 + n])
        svs = (vs_scr, lambda r0, n, b=b: vs_scr[b * SPAD + r0:b * SPAD + r0 + n, :])
        attn_core(4, kcols, sks, svs, NTOKS, sel, sel[0:NTOKS, b, :, :], qTs,
                  lambda g, b=b: qTs[0:64, 2 * g:2 * g + 2, 4 * b:4 * b + 4])
        load(at_scr, at_scr[4 * b:4 * b + 4, :], attn_o, attn_o[0:4, :], eng=PL)
    load(attn_o, attn_o[0:NTOKS, :], at_scr, at_scr[0:NTOKS, :])
    load(x_tm, x_tm[0:NTOKS, :], xs_in, xs_in[:, :])
    transpose_tm(x_tm, 0, NTOKS, 8, 128, xT, lambda c: xT[:, c, 0:NTOKS], banks=(0, 1))

    def zsrc_s(nt):
        for b in range(NSC):
            load(zs_scr, zs_scr[6 * b:6 * b + 2, :], sconv_in, sconv_in[2 * b:2 * b + 2, :], eng=PL)
            load(zs_scr, zs_scr[6 * b + 2:6 * b + 6, :], z2, z2[4 * b:4 * b + 4, :], eng=PL)
        for b in range(NSC):
            load(z0, z0[4 * b:4 * b + 4, :], zs_scr, zs_scr[6 * b:6 * b + 4, :])
            load(z1, z1[4 * b:4 * b + 4, :], zs_scr, zs_scr[6 * b + 1:6 * b + 5, :])
            load(cv_s, cv_s[2 * b:2 * b + 2, :], zs_scr, zs_scr[6 * b + 4:6 * b + 6, :], eng=PL)
    mark('s_attn')
    tail(NTOKS, xT, 0, zsrc_s, y_s, 0)
    mark('s_tail')
    import json as _json
    if cfg.get('MARKFILE'):
        _json.dump(MARK, open(cfg['MARKFILE'], 'w'))

    k.final_wait(PL, outs)
    with nc.Block() as block:
        @block.tensor
        def _(e):
            for f in PE.q:
                f(e)

        @block.scalar
        def _(e):
            for f in ACT.q:
                f(e)

        @block.vector
        def _(e):
            for f in DVE.q:
                f(e)

        @block.sync
        def _(e):
            for f in SP.q:
                f(e)

        @block.gpsimd
        def _(e):
            for f in PL.q:
                f(e)
    for cm in reversed(k.stack):
        cm.__exit__(None, None, None)
    print("instr counts", {e.name: len(e.q) for e in (PE, ACT, DVE, SP, PL)}, "sems", len(k.stack))
    return nc


def kernel(x_prompt, x_sample, cache_k, cache_v, cache_idx_k, state_conv, page_table, meta_tokens,
           w_in, b_in, w_conv, w_attn_up, w_conv_out, w_o, ln1_g, ln1_b,
           w_group, b_group, w_expert_router, b_expert_router, w_gate, w_up, w_down, ln2_g, ln2_b):
    f32 = np.float32
    A = lambda a: np.ascontiguousarray(np.asarray(a))
    x_prompt = A(x_prompt); x_sample = A(x_sample)
    SEQ = x_prompt.shape[1]
    NB = x_sample.shape[0]
    NPHYS = cache_k.shape[1]
    NPG = page_table.shape[1]
    PAST = NPG * 128
    NSC = NB // NCORES
    NQT = SEQ // 128
    NSLOT = NQT // NCORES
    NT = NQT + 1
    TP = SEQ + 16
    cfg = dict(SEQ=SEQ, NPHYS=NPHYS, NSC=NSC, NPG=NPG, KTOP_P=min(256, TP // 4), KTOP_S=min(256, (PAST + 4) // 4),
               ALPHA=float(2.0 ** 0.25), IDX_W_SCALE=float((8 ** -0.5) * (64 ** -0.5)))
    nc = build(cfg)
    xfull = np.concatenate([A(meta_tokens).astype(f32), x_prompt[0]], axis=0)
    tri = np.where(np.arange(128)[None, :] <= np.arange(128)[:, None], 0.0, NEG).astype(f32)
    ident = np.eye(128, dtype=f32)
    tri4 = np.where(np.arange(4)[None, :] <= (np.arange(128) % 4)[:, None], 0.0, NEG).astype(f32)
    invf = np.power(np.float32(10000.0), -np.arange(32, dtype=f32) * np.float32(2.0) / np.float32(64)).astype(f32)

    def cstable(pos):
        a = pos.astype(f32)[:, None] * invf[None, :]
        return np.concatenate([-np.cos(a), -np.sin(a)], axis=1).astype(f32)
    pidx = np.arange(128, dtype=f32)[:, None].copy()
    cstab_s = cstable(PAST + (np.arange(128) % 4))
    ckv = np.concatenate([A(cache_k)[0].reshape(NPHYS * 128, 256), A(cache_v)[0].reshape(NPHYS * 128, 256),
                          A(cache_idx_k)[0].reshape(NPHYS * 128, 64)], axis=1)
    common = dict(
        tri=tri, tri4=tri4, ident=ident, pidx=pidx, cstab_s=cstab_s, ckv=ckv,
        w_in=A(w_in)[0], b_in=A(b_in)[0][None, :], w_conv=A(w_conv)[0].reshape(1, 1536),
        w_a=A(w_attn_up)[0], w_b=A(w_conv_out)[0], w_o=A(w_o)[0],
        ln=np.stack([A(ln1_g)[0], A(ln1_b)[0], A(ln2_g)[0], A(ln2_b)[0]]).astype(f32),
        w_r=np.concatenate([A(w_group)[0], A(w_expert_router)[0]], axis=1),
        b_r=np.concatenate([A(b_group)[0], A(b_expert_router)[0]])[None, :],
        w_gate=A(w_gate)[0].reshape(16 * D, 256), w_up=A(w_up)[0].reshape(16 * D, 256),
        w_down=A(w_down)[0].reshape(16 * 256, D))
    in_maps = []
    for c in range(NCORES):
        perm = [c] + [r for r in range(8) if r != c]
        tiles = [8 * jb + perm[r] for jb in range(NSLOT) for r in range(8)]
        xall = np.concatenate([xfull[0:16]] + [xfull[16 + a * 128:16 + (a + 1) * 128] for a in tiles], axis=0)
        cstab = np.zeros((128, NT * 64), f32)
        cstab[:, 0:64] = cstable(np.arange(128))
        for n, a in enumerate(tiles):
            cstab[:, (n + 1) * 64:(n + 2) * 64] = cstable(16 + a * 128 + np.arange(128))
        xhalo = np.concatenate([xfull[16 + (8 * j + c) * 128 - 2:16 + (8 * j + c) * 128] for j in range(NSLOT)], axis=0)
        cmask = np.zeros((128, 1024), f32)
        cmask[:, 0:128] = tri
        for r in range(1, 8):
            if perm[r] > c:
                cmask[:, r * 128:(r + 1) * 128] = NEG
        m = dict(common)
        m.update(xall=xall, cstab=cstab, xhalo=xhalo, cmask=cmask,
                 xs=x_sample[c * NSC:(c + 1) * NSC].reshape(NSC * 4, D),
                 pt=A(page_table)[c * NSC:(c + 1) * NSC].astype(np.int32),
                 sconv=A(state_conv)[0, c * NSC:(c + 1) * NSC].reshape(NSC * 2, 512))
        in_maps.append(m)
    res = run_bass_kernel_spmd(nc, in_maps, core_ids=list(range(NCORES))).results
    y_prompt = np.zeros((1, SEQ, D), f32)
    k_prompt = np.zeros((1, 1, TP, 4, 64), f32); v_prompt = np.zeros((1, 1, TP, 4, 64), f32)
    ik_prompt = np.zeros((1, 1, TP, 64), f32)
    y_sample = np.zeros((NB, 4, D), f32)
    k_sample = np.zeros((1, NB, 4, 4, 64), f32); v_sample = np.zeros((1, NB, 4, 4, 64), f32)
    ik_sample = np.zeros((1, NB, 4, 64), f32)
    conv_sample = np.zeros((1, NB, 2, 512), f32)
    k_prompt[0, 0, 0:16] = res[0]["k_m"].reshape(16, 4, 64)
    v_prompt[0, 0, 0:16] = res[0]["v_m"].reshape(16, 4, 64)
    ik_prompt[0, 0, 0:16] = res[0]["ik_m"]
    for c in range(NCORES):
        r = res[c]
        for j in range(NSLOT):
            a = 8 * j + c
            y_prompt[0, a * 128:(a + 1) * 128] = r["y_p"][j * 128:(j + 1) * 128]
            k_prompt[0, 0, 16 + a * 128:16 + (a + 1) * 128] = r["k_p"][j * 128:(j + 1) * 128].reshape(128, 4, 64)
            v_prompt[0, 0, 16 + a * 128:16 + (a + 1) * 128] = r["v_p"][j * 128:(j + 1) * 128].reshape(128, 4, 64)
            ik_prompt[0, 0, 16 + a * 128:16 + (a + 1) * 128] = r["ik_p"][j * 128:(j + 1) * 128]
        y_sample[c * NSC:(c + 1) * NSC] = r["y_s"].reshape(NSC, 4, D)
        k_sample[0, c * NSC:(c + 1) * NSC] = r["k_s"].reshape(NSC, 4, 4, 64)
        v_sample[0, c * NSC:(c + 1) * NSC] = r["v_s"].reshape(NSC, 4, 4, 64)
        ik_sample[0, c * NSC:(c + 1) * NSC] = r["ik_s"].reshape(NSC, 4, 64)
        conv_sample[0, c * NSC:(c + 1) * NSC] = r["cv_s"].reshape(NSC, 2, 512)
    conv_prompt = res[NCORES - 1]["cv_p"].reshape(1, 1, 2, 512).astype(f32)
    return (y_prompt, y_sample, k_prompt, v_prompt, ik_prompt, conv_prompt,
            k_sample, v_sample, ik_sample, conv_sample)
```

```python
import math
import numpy as np
import concourse.bass as bass
import concourse.mybir as mybir
from concourse.bass_utils import run_bass_kernel_spmd

F32 = mybir.dt.float32
BF16 = mybir.dt.bfloat16
I32 = mybir.dt.int32
ALU = mybir.AluOpType
AF = mybir.ActivationFunctionType
AX = mybir.AxisListType

D = 1024
NCORES = 8
O_Q, O_K, O_V, O_IQ, O_IW, O_IK, O_CU, O_CB, O_CC, O_GA, O_GB, PROJ = (
    0, 512, 768, 1024, 1536, 1544, 1608, 2120, 2632, 3144, 4168, 5192)
LN_EPS = 1e-5
NEG = -1.0e30
MNEG = -30000.0
NIT = 26
ACCUM = True


class Tl:
    def __init__(self, h, name):
        self.h = h
        self.name = name
        self.w = None
        self.r = {}
        self.dsem = None
        self.dcnt = 0

    def __getitem__(self, k):
        return self.h[k]


class View:
    def __init__(self, parent, h):
        self.p = parent
        self.h = h
        self.name = parent.name

    w = property(lambda s: s.p.w, lambda s, v: setattr(s.p, "w", v))
    r = property(lambda s: s.p.r, lambda s, v: setattr(s.p, "r", v))
    dsem = property(lambda s: s.p.dsem, lambda s, v: setattr(s.p, "dsem", v))
    dcnt = property(lambda s: s.p.dcnt, lambda s, v: setattr(s.p, "dcnt", v))

    def __getitem__(self, k):
        return self.h[k]


class Eng:
    def __init__(self, name, e, sem, is_pe=False):
        self.name = name
        self.e = e
        self.sem = sem
        self.cnt = 0
        self.waited = {}
        self.is_pe = is_pe
        self.q = []


class K:
    def __init__(self, nc, cfg):
        self.nc = nc
        self.cfg = cfg
        self.stack = []
        self.dma_sems = []

    def enter(self, cm):
        v = cm.__enter__()
        self.stack.append(cm)
        return v

    def sem(self, name):
        return self.enter(self.nc.semaphore(name))

    def sb(self, name, shape, dt=F32):
        return Tl(self.enter(self.nc.sbuf_tensor("s_" + name, list(shape), dt)), "s_" + name)

    def ps(self, name, shape, dt=F32):
        return Tl(self.enter(self.nc.psum_tensor(name, list(shape), dt)), name)

    def dram(self, name, shape, dt=F32, kind="Internal"):
        return Tl(self.nc.dram_tensor(name, list(shape), dt, kind=kind).ap(), name)

    def _deps(self, eng, reads, writes):
        need = {}
        def add(sv):
            if sv is None:
                return
            s, v = sv
            if need.get(s, (None, 0))[1] < v:
                need[s] = (s, v)
        for t in reads:
            add(t.w)
        for t in writes:
            add(t.w)
            for s, v in t.r.items():
                add((s, v))
        out = []
        for s, v in need.values():
            if eng.is_pe and s is eng.sem:
                continue
            if eng.waited.get(s, 0) >= v:
                continue
            eng.waited[s] = v
            out.append((s, v))
        return out

    def op(self, eng, fn, reads=(), writes=()):
        waits = self._deps(eng, reads, writes)
        eng.cnt += 1
        c = eng.cnt
        sem = eng.sem
        def emit(e):
            for s, v in waits:
                e.wait_ge(s, v)
            fn(e).then_inc(sem, 1)
        eng.q.append(emit)
        for t in reads:
            if t.r.get(sem, 0) < c:
                t.r[sem] = c
        for t in writes:
            t.w = (sem, c)
            t.r = {}

    def dma(self, eng, fn, reads, dst):
        waits = self._deps(eng, reads, [dst])
        if dst.dsem is None:
            dst.dsem = self.sem("d_" + dst.name)
        dst.dcnt += 16
        c = dst.dcnt
        sem = dst.dsem
        def emit(e):
            for s, v in waits:
                e.wait_ge(s, v)
            fn(e).then_inc(sem, 16)
        eng.q.append(emit)
        for t in reads:
            if t.r.get(sem, 0) < c:
                t.r[sem] = c
        dst.w = (sem, c)
        dst.r = {}

    def final_wait(self, eng, tiles):
        waits = self._deps(eng, tiles, [])
        def emit(e):
            for s, v in waits:
                e.wait_ge(s, v)
        eng.q.append(emit)


def build(cfg):
    SEQ = cfg["SEQ"]; NPHYS = cfg["NPHYS"]; NSC = cfg["NSC"]; NPG = cfg["NPG"]
    KP = cfg["KTOP_P"]; KS = cfg["KTOP_S"]
    NQT = SEQ // 128
    NSLOT = NQT // NCORES
    NT = NQT + 1
    TP = SEQ + 16
    LS = NPG * 128 + 4
    NTOKS = NSC * 4
    ALPHA = cfg["ALPHA"]

    nc = bass.Bass("TRN2", target_bir_lowering=False)
    k = K(nc, cfg)

    def din(name, shape, dt=F32):
        return Tl(nc.dram_tensor(name, list(shape), dt, kind="ExternalInput").ap(), name)

    def dout(name, shape, dt=F32):
        return Tl(nc.dram_tensor(name, list(shape), dt, kind="ExternalOutput").ap(), name)

    xall = din("xall", [TP, D])
    cstab_in = din("cstab", [128, NT * 64])
    cstab_s_in = din("cstab_s", [128, 64])
    xhalo = din("xhalo", [NSLOT * 2, D])
    cmask_in = din("cmask", [128, 1024])
    tri_in = din("tri", [128, 128])
    tri4_in = din("tri4", [128, 4])
    ident_in = din("ident", [128, 128])
    pidx_in = din("pidx", [128, 1])
    xs_in = din("xs", [NTOKS, D])
    pt_in = din("pt", [NSC, NPG], I32)
    sconv_in = din("sconv", [NSC * 2, 512])
    ckv_in = din("ckv", [NPHYS * 128, 576])
    w_in = din("w_in", [D, PROJ])
    b_in = din("b_in", [1, PROJ])
    w_conv = din("w_conv", [1, 3 * 512])
    w_a = din("w_a", [512, D])
    w_b = din("w_b", [512, D])
    w_o = din("w_o", [D, D])
    ln_in = din("ln", [4, D])
    w_r = din("w_r", [D, 20])
    b_r = din("b_r", [1, 20])
    w_gate = din("w_gate", [16 * D, 256])
    w_up = din("w_up", [16 * D, 256])
    w_down = din("w_down", [16 * 256, D])
    y_p = dout("y_p", [NSLOT * 128, D])
    k_p = dout("k_p", [NSLOT * 128, 256])
    v_p = dout("v_p", [NSLOT * 128, 256])
    ik_p = dout("ik_p", [NSLOT * 128, 64])
    k_m = dout("k_m", [16, 256])
    v_m = dout("v_m", [16, 256])
    ik_m = dout("ik_m", [16, 64])
    cv_p = dout("cv_p", [2, 512])
    y_s = dout("y_s", [NTOKS, D])
    k_s = dout("k_s", [NTOKS, 256])
    v_s = dout("v_s", [NTOKS, 256])
    ik_s = dout("ik_s", [NTOKS, 64])
    cv_s = dout("cv_s", [NSC * 2, 512])
    outs = [y_p, k_p, v_p, ik_p, k_m, v_m, ik_m, cv_p, y_s, k_s, v_s, ik_s, cv_s]
    KPAD = NT * 128
    kT_scr = k.dram("kT_scr", [64, 4, KPAD], BF16)
    ikT_scr = k.dram("ikT_scr", [64, KPAD], BF16)
    v_scr = k.dram("v_scr", [NT * 128, 260], BF16)
    SPAD = (NPG + 1) * 128
    kTs_scr = k.dram("kTs_scr", [NSC * 64, 4, SPAD], BF16)
    ikTs_scr = k.dram("ikTs_scr", [NSC * 64, SPAD], BF16)
    vs_scr = k.dram("vs_scr", [NSC * SPAD, 260], BF16)
    win_bf = k.dram("win_bf", [D, PROJ], BF16)
    wa_bf = k.dram("wa_bf", [512, D], BF16)
    wb_bf = k.dram("wb_bf", [512, D], BF16)
    wo_bf = k.dram("wo_bf", [D, D], BF16)
    wgu_bf = k.dram("wgu_bf", [16 * D, 512], BF16)
    wd_bf = k.dram("wd_bf", [16 * 256, D], BF16)
    z_scr = k.dram("z_scr", [130, 512], F32)
    zs_scr = k.dram("zs_scr", [NSC * 6, 512], F32)
    at_scr = k.dram("at_scr", [128, 512], F32)
    iw_scr = k.dram("iw_scr", [128, 8], F32)

    PE = Eng("pe", nc.tensor, k.sem("s_pe"), is_pe=True)
    ACT = Eng("act", nc.scalar, k.sem("s_act"))
    DVE = Eng("dve", nc.vector, k.sem("s_dve"))
    SP = Eng("sp", nc.sync, k.sem("s_sp"))
    PL = Eng("pl", nc.gpsimd, k.sem("s_pl"))

    ident = k.sb("ident", [128, 128])
    identb = k.sb("identb", [128, 128], BF16)
    idrep = k.sb("idrep", [128, 512], BF16)
    pidx = k.sb("pidx_sb", [128, 1])
    bsl = [k.sb("bsl0", [128, 512]), k.sb("bsl1", [128, 512])]
    ln_bc = k.sb("ln_bc", [128, 2 * D])
    wconv_bc = k.sb("wconv_bc", [128, 3 * 512])
    br_bc = k.sb("br_bc", [128, 20])
    wr_sb = k.sb("wr_sb", [128, 8, 20])
    Ibuf = k.sb("Ibuf", [128, max(KPAD, PROJ)])
    Bm = k.sb("Bm", [128, max(KPAD, PROJ)], BF16)
    x_tm = k.sb("x_tm", [128, D])
    hT_f = View(x_tm, x_tm.h[:, :].rearrange("p (c q) -> p c q", q=128))
    xT = k.sb("xT", [128, 8, 128], BF16)
    xTh = k.sb("xTh", [128, 8, 2], BF16)
    wbuf = [k.sb("wbuf0", [128, 8, 512], BF16), k.sb("wbuf1", [128, 8, 512], BF16)]
    wdbuf = k.sb("wdbuf", [128, 2, D], BF16)
    pa = k.sb("pa", [128, 512])
    pb = k.sb("pb", [128, 512])
    pc = k.sb("pc", [128, 512])
    pd = k.sb("pd", [128, 512])
    pe_ = k.sb("pe_t", [128, 512])
    cs = k.sb("cs", [128, 2, 32])
    ang = k.sb("ang", [128, 32])
    ang2 = k.sb("ang2", [128, 32])
    qT2 = k.sb("qT2", [64, 8, 128], BF16)
    iqT2 = k.sb("iqT2", [64, 8, 128], BF16)
    iw_sb = k.sb("iw_sb", [128, 8])
    kst = k.sb("kst", [64, 4, 128], BF16)
    ikst = k.sb("ikst", [64, 128], BF16)
    vst = k.sb("vst", [128, 4, 65], BF16)
    ikc = [k.sb("ikc0", [64, 512], BF16), k.sb("ikc1", [64, 512], BF16)]
    rl = [k.sb("rl0", [128, 512]), k.sb("rl1", [128, 512])]
    pT = [k.sb("pT0", [128, 8, 128], BF16), k.sb("pT1", [128, 8, 128], BF16)]
    o_sb = k.sb("o_sb", [128, 8, 65])
    rcp = k.sb("rcp", [128, 8])
    attn_o = k.sb("attn_o", [128, 512])
    sm = k.sb("sm", [128, 64])
    aT = k.sb("aT", [128, 4, 128], BF16)
    cT = k.sb("cT", [128, 4, 128], BF16)
    mT = k.sb("mT", [128, 8, 128], BF16)
    hT_b = k.sb("hT_b", [128, 8, 128], BF16)
    actT = k.sb("actT", [128, 2, 128], BF16)
    h1 = k.sb("h1", [128, D])
    r_tm = k.sb("r_tm", [128, D])
    m_tm = r_tm
    z0 = pd; z1 = pe_; z2 = k.sb("z2", [128, 512])
    lg = k.sb("lg", [128, 20])
    rt = k.sb("rt", [128, 64])
    comb = k.sb("comb", [128, 16])
    pt_i = k.sb("pt_i", [128, NPG], I32)
    pt_f = k.sb("pt_f", [128, NPG])
    rows_i = k.sb("rows_i", [128, NPG], I32)
    NPB = cfg.get("NPB", 2)
    pgset = []
    for i in range(NPB):
        pgset.append((k.sb("pgkv%d" % i, [128, 576]), None, None,
                      k.sb("ksts%d" % i, [64, 4, 128], BF16), k.sb("iksts%d" % i, [64, 128], BF16), k.sb("vsts%d" % i, [128, 4, 65], BF16)))
    rows_l = [rows_i, k.sb("rows_i2", [128, NPG], I32)]
    tri4 = k.sb("tri4_sb", [128, 4])
    oh = k.sb("oh", [128, NSC])
    iwm = k.sb("iwm", [128, NSC, 8])
    sel = k.sb("sel", [128, NSC, 4, 4], BF16)
    B = [k.ps("B%d" % i, [128, 512]) for i in range(8)]

    def load(dst, dst_ap, src, src_ap, eng=SP, **kw):
        k.dma(eng, lambda e: e.dma_start(out=dst_ap, in_=src_ap, **kw), [src], dst)

    def dve(fn, reads, writes):
        k.op(DVE, fn, reads, writes)

    def act(fn, reads, writes):
        k.op(ACT, fn, reads, writes)

    def mm(out_t, out_ap, l_t, l_ap, r_t, r_ap, start, stop, skip=True):
        k.op(PE, lambda e: e.matmul(out_ap, l_ap, r_ap, start=start, stop=stop, skip_group_check=skip),
             [l_t, r_t], [out_t])

    def tr(out_t, out_ap, in_t, in_ap, npart):
        k.op(PE, lambda e: e.transpose(out_ap, in_ap, ident[0:npart, 0:npart]), [in_t, ident], [out_t])

    load(ident, ident[:, :], ident_in, ident_in[:, :])
    load(pidx, pidx[:, :], pidx_in, pidx_in[:, :])
    load(wconv_bc, wconv_bc[:, :], w_conv, w_conv[0:1, :].partition_broadcast(128))
    load(br_bc, br_bc[:, :], b_r, b_r[0:1, :].partition_broadcast(128))
    load(wr_sb, wr_sb[:, :, :], w_r, w_r[:, :].rearrange("(c p) n -> p c n", p=128))
    dve(lambda e: e.tensor_copy(identb[:, :], ident[:, :]), [ident], [identb])
    for r in range(4):
        dve(lambda e, r=r: e.tensor_copy(idrep[:, r * 128:(r + 1) * 128], ident[:, :]), [ident], [idrep])
    dve(lambda e: e.memset(vst[:, :, :], 1.0), [], [vst])
    for st in pgset:
        dve(lambda e, st=st: e.memset(st[5][:, :, :], 1.0), [], [st[5]])

    WST = max(KPAD, PROJ)
    nreg = max(1, min(3, WST // PROJ))
    stg_f = [Tl(Ibuf.h[:, i * PROJ:(i + 1) * PROJ], "stgf%d" % i) for i in range(nreg)]
    stg_b = [Tl(Bm.h[:, i * PROJ:(i + 1) * PROJ], "stgb%d" % i) for i in range(nreg)]
    st_i = [0]

    def conv_w(src, dst, rows, cols, dst_col0=0):
        nb = max(1, PROJ // cols)
        r0 = 0
        while r0 < rows:
            nbk = min(nb, (rows - r0) // 128)
            sf = stg_f[st_i[0] % nreg]; sbb = stg_b[st_i[0] % nreg]
            st_i[0] += 1
            load(sf, sf[:, 0:nbk * cols].rearrange("p (c n) -> p c n", n=cols), src,
                 src[r0:r0 + nbk * 128, 0:cols].rearrange("(c p) n -> p c n", p=128))
            dve(lambda e, sf=sf, sbb=sbb, w=nbk * cols: e.tensor_copy(sbb[:, 0:w], sf[:, 0:w]), [sf], [sbb])
            load(dst, dst[r0:r0 + nbk * 128, dst_col0:dst_col0 + cols].rearrange("(c p) n -> p c n", p=128), sbb,
                 sbb[:, 0:nbk * cols].rearrange("p (c n) -> p c n", n=cols), eng=PL)
            r0 += nbk * 128

    conv_w(w_in, win_bf, D, PROJ)
    conv_w(w_a, wa_bf, 512, D)
    conv_w(w_b, wb_bf, 512, D)
    conv_w(w_o, wo_bf, D, D)
    conv_w(w_gate, wgu_bf, 16 * D, 256, 0)
    conv_w(w_up, wgu_bf, 16 * D, 256, 256)
    conv_w(w_down, wd_bf, 16 * 256, D)
    dve(lambda e: e.memset(Ibuf[:, 0:1], 0.0), [], stg_f + [Ibuf])
    dve(lambda e: e.memset(Bm[:, 0:1], 0.0), [], stg_b + [Bm])
    wkv_v = View(Bm, Bm.h[:, 0:4096].rearrange("p (c n) -> p c n", n=512))
    wik_v = View(Bm, Bm.h[:, 4096:4608].rearrange("p (c n) -> p c n", n=64))
    load(wkv_v, wkv_v[:, :, :], win_bf, win_bf[0:D, O_K:O_K + 512].rearrange("(c p) n -> p c n", p=128))
    load(wik_v, wik_v[:, :, :], win_bf, win_bf[0:D, O_IK:O_IK + 64].rearrange("(c p) n -> p c n", p=128))

    wdbuf2 = View(Ibuf, Ibuf.h[:, 0:1024].bitcast(BF16).rearrange("p (c n) -> p c n", n=1024))
    wb_i = [0]
    bs_i = [0]

    def add_bias(dst_t, dst_ap, src_t, src_ap, nrow, off, n):
        bt = bsl[bs_i[0] % 2]
        bs_i[0] += 1
        load(bt, bt[:, 0:n], b_in, b_in[0:1, off:off + n].partition_broadcast(128))
        dve(lambda e: e.tensor_tensor(dst_ap, src_ap, bt[0:nrow, 0:n], op=ALU.add), [src_t, bt], [dst_t])

    def lin(xT_t, nt, kc, wsrc, row0, col0, ncols, out_t, out_ap, tok0=0, first=True, last=True):
        wbt = wbuf[wb_i[0] % 2]
        wb_i[0] += 1
        load(wbt, wbt[:, 0:kc, 0:ncols], wsrc,
             wsrc[row0:row0 + kc * 128, col0:col0 + ncols].rearrange("(c p) n -> p c n", p=128))
        for c in range(kc):
            mm(out_t, out_ap, xT_t, xT_t[:, c, tok0:tok0 + nt], wbt, wbt[:, c, 0:ncols],
               start=(first and c == 0), stop=(last and c == kc - 1))

    def transpose_tm(src_t, src_cols, nt, ncol_chunks, csz, dst_t, dst_fn, banks=(0, 1)):
        per = 512 // 128
        c = 0
        bi = 0
        while c < ncol_chunks:
            n = min(per, ncol_chunks - c)
            bk = B[banks[bi % len(banks)]]
            bi += 1
            for i in range(n):
                tr(bk, bk[0:csz, i * 128:i * 128 + nt], src_t,
                   src_t[0:nt, src_cols + (c + i) * csz: src_cols + (c + i + 1) * csz], nt)
            for i in range(n):
                dve(lambda e, i=i, c=c, bk=bk: e.tensor_copy(dst_fn(c + i), bk[0:csz, i * 128:i * 128 + nt]),
                    [bk], [dst_t])
            c += n

    def rope_tables(n):
        if n == "s":
            load(cs, cs[:, :, :].rearrange("p a d -> p (a d)"), cstab_s_in, cstab_s_in[:, :])
        else:
            load(cs, cs[:, :, :].rearrange("p a d -> p (a d)"), cstab_in, cstab_in[:, n * 64:(n + 1) * 64])

    def rope(src_t, src_c0, nh, nt, dst_t, dst_c0, t1, t2):
        def v(t, c0, half):
            return t[0:nt, c0:c0 + nh * 64].rearrange("p (h d) -> p h d", d=64)[:, :, half * 32:(half + 1) * 32]
        mc = cs[0:nt, 0:1, :].to_broadcast([nt, nh, 32])
        ms = cs[0:nt, 1:2, :].to_broadcast([nt, nh, 32])
        w1 = t1[0:nt, 0:nh * 32].rearrange("p (h d) -> p h d", d=32)
        w2 = t2[0:nt, 0:nh * 32].rearrange("p (h d) -> p h d", d=32)
        x1 = v(src_t, src_c0, 0); x2 = v(src_t, src_c0, 1)
        o1 = v(dst_t, dst_c0, 0); o2 = v(dst_t, dst_c0, 1)
        dve(lambda e: e.tensor_tensor(w1, x2, ms, op=ALU.mult), [src_t, cs], [t1])
        dve(lambda e: e.tensor_tensor(w2, x1, mc, op=ALU.mult), [src_t, cs], [t2])
        dve(lambda e: e.tensor_tensor(o1, w1, w2, op=ALU.subtract), [t1, t2], [dst_t])
        dve(lambda e: e.tensor_tensor(w1, x1, ms, op=ALU.mult), [src_t, cs], [t1])
        dve(lambda e: e.tensor_tensor(w2, x2, mc, op=ALU.mult), [src_t, cs], [t2])
        dve(lambda e: e.scalar_tensor_tensor(o2, w1, -1.0, w2, op0=ALU.mult, op1=ALU.subtract), [t1, t2], [dst_t])

    def load_xT(src, row0, nt, dst_x, dst_xT):
        load(dst_x, dst_x[0:nt, :], src, src[row0:row0 + nt, :])
        transpose_tm(dst_x, 0, nt, 8, 128, dst_xT, lambda c: dst_xT[:, c, 0:nt], banks=(0, 1))

    def kvik_tile(xT_t, nt, pos_ap, kout, vout, ikout, orow0, kT_dst, ikT_dst, v_dst, kcol0, vrow0, do_out, ws=None):
        ta, tb, tc, tk, tik, tv = ws if ws is not None else (pa, pb, pc, kst, ikst, vst)
        for c in range(8):
            mm(B[2], B[2][0:nt, 0:512], xT_t, xT_t[:, c, 0:nt], wkv_v, wkv_v[:, c, :], c == 0, c == 7)
        for c in range(8):
            mm(B[3], B[3][0:nt, 0:64], xT_t, xT_t[:, c, 0:nt], wik_v, wik_v[:, c, :], c == 0, c == 7)
        add_bias(ta, ta[0:nt, :], B[2], B[2][0:nt, :], nt, O_K, 512)
        add_bias(tb, tb[0:nt, 0:64], B[3], B[3][0:nt, 0:64], nt, O_IK, 64)
        rope_tables(pos_ap)
        rope(ta, 0, 4, nt, tc, 0, pd, pe_)
        rope(tb, 0, 1, nt, tc, 256, pd, pe_)
        if do_out:
            load(kout, kout[orow0:orow0 + nt, :], tc, tc[0:nt, 0:256], eng=PL)
            load(vout, vout[orow0:orow0 + nt, :], ta, ta[0:nt, 256:512], eng=PL)
            load(ikout, ikout[orow0:orow0 + nt, :], tc, tc[0:nt, 256:320], eng=PL)
        transpose_tm(tc, 0, nt, 4, 64, tk, lambda c: tk[:, c, 0:nt], banks=(4, 5))
        transpose_tm(tc, 256, nt, 1, 64, tik, lambda c: tik[:, 0:nt], banks=(4, 5))
        dve(lambda e: e.tensor_copy(tv[0:nt, :, 0:64], ta[0:nt, 256:512].rearrange("p (h d) -> p h d", d=64)), [ta], [tv])
        load(kT_dst[0], kT_dst[1](kcol0, nt), tk, tk[:, :, 0:nt], eng=PL)
        load(ikT_dst[0], ikT_dst[1](kcol0, nt), tik, tik[:, 0:nt], eng=PL)
        load(v_dst[0], v_dst[1](vrow0, nt), tv, tv[0:nt, :, :].rearrange("p h d -> p (h d)"), eng=PL)

    MARK = []
    def mark(name):
        MARK.append((name, PE.cnt, DVE.cnt, ACT.cnt))
    mark('phase0_end')
    pk = (kT_scr, lambda c0, n: kT_scr[:, :, c0:c0 + n])
    pik = (ikT_scr, lambda c0, n: ikT_scr[:, c0:c0 + n])
    pv = (v_scr, lambda r0, n: v_scr[r0:r0 + n, :])
    for n in range(NT):
        nt = 16 if n == 0 else 128
        row0 = 0 if n == 0 else 16 + (n - 1) * 128
        xb, xTb = ((x_tm, xT), (h1, mT))[n % 2]
        wsA = ((pa, pb, pc, kst, ikst, vst), (rl[0], rl[1], attn_o, pgset[0][3], pgset[0][4], pgset[0][5]))[n % 2]
        load_xT(xall, row0, nt, xb, xTb)
        own = (n == 0) or ((n - 1) % 8 == 0)
        if n == 0:
            kvik_tile(xTb, nt, 0, k_m, v_m, ik_m, 0, pk, pik, pv, 0, 0, True, ws=wsA)
        else:
            slot = (n - 1) // 8
            kvik_tile(xTb, nt, n, k_p, v_p, ik_p, slot * 128, pk, pik, pv,
                      16 + (n - 1) * 128, 16 + (n - 1) * 128, own, ws=wsA)

    mark('phaseA_end')
    def indexer(nrow, nkeys, iksrc, iq_t, w_t, w_fn, first):
        ci = 0
        for c0 in range(0, nkeys, 512):
            cn = min(512, nkeys - c0)
            ib = ikc[ci % 2]
            ci += 1
            load(ib, ib[:, 0:cn], iksrc[0], iksrc[1](c0, cn))
            for h in range(8):
                bk = B[h % 2]
                mm(bk, bk[0:nrow, 0:cn], iq_t, iq_t[0:64, h, 0:nrow], ib, ib[:, 0:cn], True, True)
                r = rl[h % 2]
                act(lambda e, r=r, bk=bk, cn=cn: e.activation(r[0:nrow, 0:cn], bk[0:nrow, 0:cn], AF.Relu), [bk], [r])
                w_ap = w_fn(h)
                if first and h == 0:
                    dve(lambda e, r=r, c0=c0, cn=cn, w_ap=w_ap: e.tensor_scalar(Ibuf[0:nrow, c0:c0 + cn], r[0:nrow, 0:cn], w_ap, None, op0=ALU.mult), [r, w_t], [Ibuf])
                else:
                    dve(lambda e, r=r, c0=c0, cn=cn, w_ap=w_ap: e.scalar_tensor_tensor(Ibuf[0:nrow, c0:c0 + cn], r[0:nrow, 0:cn], w_ap, Ibuf[0:nrow, c0:c0 + cn], op0=ALU.mult, op1=ALU.add), [r, w_t, Ibuf], [Ibuf])

    def threshold(nq, nkeys, ktop, mask_fn):
        A_ = sm[0:nq, 0:1]; lo = sm[0:nq, 1:2]; hi = sm[0:nq, 2:3]; mid = sm[0:nq, 3:4]
        cnt = sm[0:nq, 4:5]; prd = sm[0:nq, 5:6]; tmp = sm[0:nq, 6:7]
        dve(lambda e: e.tensor_reduce(A_, Ibuf[0:nq, 0:nkeys], axis=AX.X, op=ALU.max), [Ibuf], [sm])
        dve(lambda e: e.tensor_reduce(tmp, Ibuf[0:nq, 0:nkeys], axis=AX.X, op=ALU.min), [Ibuf], [sm])
        dve(lambda e: e.tensor_scalar(tmp, tmp, -1.0, None, op0=ALU.mult), [sm], [sm])
        dve(lambda e: e.tensor_tensor(A_, A_, tmp, op=ALU.max), [sm], [sm])
        mask_fn(nq, nkeys)
        dve(lambda e: e.tensor_scalar(hi, A_, 1.001, 1e-20, op0=ALU.mult, op1=ALU.add), [sm], [sm])
        dve(lambda e: e.tensor_scalar(lo, hi, -1.0, None, op0=ALU.mult), [sm], [sm])
        for it in range(NIT):
            sc = 2.0 ** (-it)
            dve(lambda e, sc=sc: e.scalar_tensor_tensor(mid, hi, sc, lo, op0=ALU.mult, op1=ALU.add), [sm], [sm])
            dve(lambda e: e.memset(cnt, 0.0), [], [sm])
            dve(lambda e: e.tensor_scalar(Bm[0:nq, 0:nkeys], Ibuf[0:nq, 0:nkeys], mid, 0.0, op0=ALU.is_ge, op1=ALU.add, accum_out=cnt), [Ibuf, sm], [Bm, sm])
            dve(lambda e: e.tensor_tensor(tmp, mid, lo, op=ALU.subtract), [sm], [sm])
            dve(lambda e: e.scalar_tensor_tensor(prd, cnt, float(ktop) - 0.5, tmp, op0=ALU.is_ge, op1=ALU.mult), [sm], [sm])
            dve(lambda e: e.tensor_tensor(lo, lo, prd, op=ALU.add), [sm], [sm])
        dve(lambda e: e.tensor_scalar(Bm[0:nq, 0:nkeys], Ibuf[0:nq, 0:nkeys], lo, MNEG, op0=ALU.is_lt, op1=ALU.mult), [Ibuf, sm], [Bm])

    KBLK = 7
    kbv = [View(wbuf[i], wbuf[i].h[0:64, :, :].rearrange("p c n -> p (c n)")[:, 0:4 * KBLK * 128].rearrange("p (g s) -> p g s", g=4))
           for i in range(2)]
    vbv = [View(t, t.h[:, :].bitcast(BF16)[:, 0:KBLK * 260].rearrange("p (t d) -> p t d", d=260)) for t in (h1, r_tm)]

    def attn_core(nq, kcols, ksrc, vsrc, mrow, mr_t, mrhs_ap, q_t, q_fn):
        blocks = []
        for (c0, nk) in kcols:
            if nk == 128 and blocks and blocks[-1][-1][1] == 128 and len(blocks[-1]) < KBLK:
                blocks[-1].append((c0, nk))
            else:
                blocks.append([(c0, nk)])

        def load_block(bi):
            blk = blocks[bi]
            kb = kbv[bi % 2]; vb = vbv[bi % 2]
            c0 = blk[0][0]
            n = sum(x[1] for x in blk)
            load(kb, kb[:, :, 0:n], ksrc[0], ksrc[1](c0, n))
            if blk[0][1] == 128:
                load(vb, vb[:, 0:len(blk), :], vsrc[0], vsrc[1](c0, n).rearrange("(t p) d -> p t d", p=128))
            else:
                load(vb, vb[0:n, 0, :], vsrc[0], vsrc[1](c0, n))

        nkt = len(kcols)
        tiles = []
        for bi, blk in enumerate(blocks):
            for tl, (c0, nk) in enumerate(blk):
                tiles.append((bi, tl, c0, nk, c0 - blk[0][0]))

        def scores(ti):
            bi, tl, c0, nk, off = tiles[ti]
            kb = kbv[bi % 2]
            s0 = B[2 + 2 * (ti % 2)]; s1 = B[3 + 2 * (ti % 2)]
            for sbk in (s0, s1):
                mm(sbk, sbk[0:nk, 0:4 * nq].rearrange("p (r q) -> p r q", q=nq), Bm, Bm[0:mrow, c0:c0 + nk], mr_t, mrhs_ap, True, False)
            for g in range(4):
                sbk = (s0, s1)[g // 2]
                mm(sbk, sbk[0:nk, (g % 2) * 2 * nq:(g % 2) * 2 * nq + 2 * nq].rearrange("p (r q) -> p r q", q=nq), kb, kb[:, g, off:off + nk],
                   q_t, q_fn(g), False, g % 2 == 1)
            p = pT[ti % 2]
            for bj, sbk in enumerate((s0, s1)):
                act(lambda e, bj=bj, sbk=sbk, p=p, nk=nk: e.activation(
                    p[0:nk, 4 * bj:4 * bj + 4, 0:nq], sbk[0:nk, 0:4 * nq].rearrange("p (h q) -> p h q", q=nq), AF.Exp, scale=0.125),
                    [sbk], [p])

        def pv(ti):
            bi, tl, c0, nk, off = tiles[ti]
            vb = vbv[bi % 2]
            p = pT[ti % 2]
            for h in range(8):
                ob = B[6 + h // 4]
                mm(ob, ob[0:nq, (h % 4) * 65:(h % 4) * 65 + 65], p, p[0:nk, h, 0:nq], vb,
                   vb[0:nk, tl, (h // 2) * 65:(h // 2) * 65 + 65], (ti == 0 and h % 4 == 0), ti == nkt - 1)

        load_block(0)
        if len(blocks) > 1:
            load_block(1)
        for ti in range(nkt):
            scores(ti)
            if ti >= 1:
                pv(ti - 1)
                if tiles[ti - 1][0] != tiles[ti][0] and tiles[ti][0] + 1 < len(blocks):
                    load_block(tiles[ti][0] + 1)
        pv(nkt - 1)
        for hb in range(2):
            dve(lambda e, hb=hb: e.tensor_copy(o_sb[0:nq, 4 * hb:4 * hb + 4, :], B[6 + hb][0:nq, 0:260].rearrange("p (h d) -> p h d", d=65)), [B[6 + hb]], [o_sb])
        dve(lambda e: e.reciprocal(rcp[0:nq, :], o_sb[0:nq, :, 64]), [o_sb], [rcp])
        dve(lambda e: e.tensor_tensor(attn_o[0:nq, :].rearrange("p (h d) -> p h d", d=64), o_sb[0:nq, :, 0:64],
                                      rcp[0:nq, :].unsqueeze(2).to_broadcast([nq, 8, 64]), op=ALU.mult), [o_sb, rcp], [attn_o])

    def q_project(xT_t, nt, tok0, pos_ap):
        lin(xT_t, nt, 8, win_bf, 0, O_Q, 512, B[2], B[2][0:nt, 0:512], tok0=tok0)
        lin(xT_t, nt, 8, win_bf, 0, O_IQ, 512, B[3], B[3][0:nt, 0:512], tok0=tok0)
        lin(xT_t, nt, 8, win_bf, 0, O_IW, 8, B[4], B[4][0:nt, 0:8], tok0=tok0)
        add_bias(pa, pa[0:nt, :], B[2], B[2][0:nt, :], nt, O_Q, 512)
        add_bias(pb, pb[0:nt, :], B[3], B[3][0:nt, :], nt, O_IQ, 512)
        add_bias(iw_sb, iw_sb[0:nt, :], B[4], B[4][0:nt, 0:8], nt, O_IW, 8)
        dve(lambda e: e.tensor_scalar(iw_sb[0:nt, :], iw_sb[0:nt, :], cfg["IDX_W_SCALE"], None, op0=ALU.mult), [iw_sb], [iw_sb])
        rope_tables(pos_ap)
        rope(pa, 0, 8, nt, pc, 0, pd, pe_)
        transpose_tm(pc, 0, nt, 8, 64, qT2, lambda c: qT2[:, c, 0:nt], banks=(4, 5))
        rope(pb, 0, 8, nt, pc, 0, pd, pe_)
        transpose_tm(pc, 0, nt, 8, 64, iqT2, lambda c: iqT2[:, c, 0:nt], banks=(4, 5))

    def layer_norm(src, nt, which, dst):
        for a in range(2):
            load(ln_bc, ln_bc[:, a * D:(a + 1) * D], ln_in, ln_in[2 * which + a:2 * which + a + 1, :].partition_broadcast(128))
        g_ap = ln_bc[0:nt, 0:D]
        b_ap = ln_bc[0:nt, D:2 * D]
        s = sm[0:nt, 8:9]; ss = sm[0:nt, 9:10]
        dve(lambda e: e.reduce_sum(s, src[0:nt, :], axis=AX.X), [src], [sm])
        dve(lambda e: e.tensor_scalar(s, s, -1.0 / D, None, op0=ALU.mult), [sm], [sm])
        dve(lambda e: e.tensor_scalar(src[0:nt, :], src[0:nt, :], s, None, op0=ALU.add), [src, sm], [src])
        act(lambda e: e.activation(dst[0:nt, :], src[0:nt, :], AF.Square, accum_out=ss), [src], [dst, sm])
        dve(lambda e: e.tensor_scalar(ss, ss, 1.0 / D, LN_EPS, op0=ALU.mult, op1=ALU.add), [sm], [sm])
        act(lambda e: e.activation(ss, ss, AF.Sqrt), [sm], [sm])
        dve(lambda e: e.reciprocal(ss, ss), [sm], [sm])
        dve(lambda e: e.tensor_scalar(src[0:nt, :], src[0:nt, :], ss, None, op0=ALU.mult), [src, sm], [src])
        dve(lambda e: e.tensor_tensor(src[0:nt, :], src[0:nt, :], g_ap, op=ALU.mult), [src, ln_bc], [src])
        dve(lambda e: e.tensor_tensor(dst[0:nt, :], src[0:nt, :], b_ap, op=ALU.add), [src, ln_bc], [dst])

    def tail(nt, xT_t, tok0, zsrc_fn, y_out, yrow0):
        lin(xT_t, nt, 8, win_bf, 0, O_CU, 512, B[2], B[2][0:nt, :], tok0=tok0)
        lin(xT_t, nt, 8, win_bf, 0, O_CC, 512, B[3], B[3][0:nt, :], tok0=tok0)
        lin(xT_t, nt, 8, win_bf, 0, O_CB, 512, B[4], B[4][0:nt, :], tok0=tok0)
        add_bias(pa, pa[0:nt, :], B[2], B[2][0:nt, :], nt, O_CU, 512)
        add_bias(pb, pb[0:nt, :], B[3], B[3][0:nt, :], nt, O_CC, 512)
        dve(lambda e: e.tensor_tensor(z2[0:nt, :], pa[0:nt, :], pb[0:nt, :], op=ALU.mult), [pa, pb], [z2])
        zsrc_fn(nt)
        dve(lambda e: e.tensor_tensor(pa[0:nt, :], z0[0:nt, :], wconv_bc[0:nt, 0:512], op=ALU.mult), [z0, wconv_bc], [pa])
        dve(lambda e: e.tensor_tensor(pb[0:nt, :], z1[0:nt, :], wconv_bc[0:nt, 512:1024], op=ALU.mult), [z1, wconv_bc], [pb])
        dve(lambda e: e.tensor_tensor(pa[0:nt, :], pa[0:nt, :], pb[0:nt, :], op=ALU.add), [pa, pb], [pa])
        dve(lambda e: e.tensor_tensor(pb[0:nt, :], z2[0:nt, :], wconv_bc[0:nt, 1024:1536], op=ALU.mult), [z2, wconv_bc], [pb])
        dve(lambda e: e.tensor_tensor(pa[0:nt, :], pa[0:nt, :], pb[0:nt, :], op=ALU.add), [pa, pb], [pa])
        add_bias(pb, pb[0:nt, :], B[4], B[4][0:nt, :], nt, O_CB, 512)
        dve(lambda e: e.tensor_tensor(pc[0:nt, :], pa[0:nt, :], pb[0:nt, :], op=ALU.mult), [pa, pb], [pc])
        transpose_tm(pc, 0, nt, 4, 128, cT, lambda c: cT[:, c, 0:nt], banks=(0, 1))
        transpose_tm(attn_o, 0, nt, 4, 128, aT, lambda c: aT[:, c, 0:nt], banks=(0, 1))
        for half in range(2):
            c0 = half * 512
            lin(aT, nt, 4, wa_bf, 0, c0, 512, B[2], B[2][0:nt, :])
            lin(cT, nt, 4, wb_bf, 0, c0, 512, B[3], B[3][0:nt, :])
            lin(xT_t, nt, 8, win_bf, 0, O_GA + c0, 512, B[4], B[4][0:nt, :], tok0=tok0)
            lin(xT_t, nt, 8, win_bf, 0, O_GB + c0, 512, B[5], B[5][0:nt, :], tok0=tok0)
            add_bias(pa, pa[0:nt, :], B[4], B[4][0:nt, :], nt, O_GA + c0, 512)
            add_bias(pb, pb[0:nt, :], B[5], B[5][0:nt, :], nt, O_GB + c0, 512)
            act(lambda e: e.activation(pa[0:nt, :], pa[0:nt, :], AF.Sigmoid), [pa], [pa])
            act(lambda e: e.activation(pb[0:nt, :], pb[0:nt, :], AF.Sigmoid), [pb], [pb])
            dve(lambda e: e.tensor_tensor(pa[0:nt, :], pa[0:nt, :], B[2][0:nt, :], op=ALU.mult), [pa, B[2]], [pa])
            dve(lambda e: e.tensor_tensor(pb[0:nt, :], pb[0:nt, :], B[3][0:nt, :], op=ALU.mult), [pb, B[3]], [pb])
            dve(lambda e, c0=c0: e.tensor_tensor(m_tm[0:nt, c0:c0 + 512], pa[0:nt, :], pb[0:nt, :], op=ALU.add), [pa, pb], [m_tm])
        transpose_tm(m_tm, 0, nt, 8, 128, mT, lambda c: mT[:, c, 0:nt], banks=(0, 1))
        for half in range(2):
            c0 = half * 512
            lin(mT, nt, 8, wo_bf, 0, c0, 512, B[2 + half], B[2 + half][0:nt, :])
            dve(lambda e, c0=c0, half=half: e.scalar_tensor_tensor(r_tm[0:nt, c0:c0 + 512], x_tm[0:nt, c0:c0 + 512], ALPHA, B[2 + half][0:nt, :], op0=ALU.mult, op1=ALU.add), [x_tm, B[2 + half]], [r_tm])
        layer_norm(r_tm, nt, 0, h1)
        transpose_tm(h1, 0, nt, 8, 128, hT_f, lambda c: hT_f[:, c, 0:nt], banks=(0, 1))
        dve(lambda e: e.tensor_copy(hT_b[:, :, 0:nt], hT_f[:, :, 0:nt]), [hT_f], [hT_b])
        for c in range(8):
            mm(B[2], B[2][0:nt, 0:20], hT_f, hT_f[:, c, 0:nt], wr_sb, wr_sb[:, c, :], c == 0, c == 7)
        dve(lambda e: e.tensor_tensor(lg[0:nt, :], B[2][0:nt, 0:20], br_bc[0:nt, :], op=ALU.add), [B[2], br_bc], [lg])
        R = lambda a, b: rt[0:nt, a:b]
        gl = lg[0:nt, 0:4]
        dve(lambda e: e.tensor_reduce(R(0, 1), gl, axis=AX.X, op=ALU.max), [lg], [rt])
        dve(lambda e: e.tensor_scalar(R(1, 5), gl, R(0, 1), None, op0=ALU.is_ge), [lg, rt], [rt])
        dve(lambda e: e.tensor_scalar(R(5, 9), gl, R(0, 1), None, op0=ALU.subtract), [lg, rt], [rt])
        act(lambda e: e.activation(R(5, 9), R(5, 9), AF.Exp, accum_out=R(9, 10)), [rt], [rt])
        dve(lambda e: e.reciprocal(R(10, 11), R(9, 10)), [rt], [rt])
        dve(lambda e: e.tensor_tensor(R(16, 32).rearrange("p (g j) -> p g j", j=4), lg[0:nt, 4:20].rearrange("p (g j) -> p g j", j=4),
                                      R(1, 5).unsqueeze(2).to_broadcast([nt, 4, 4]), op=ALU.mult), [lg, rt], [rt])
        dve(lambda e: e.tensor_reduce(R(32, 36), R(16, 32).rearrange("p (g j) -> p j g", j=4), axis=AX.X, op=ALU.add), [rt], [rt])
        dve(lambda e: e.tensor_reduce(R(36, 37), R(32, 36), axis=AX.X, op=ALU.max), [rt], [rt])
        dve(lambda e: e.tensor_scalar(R(37, 41), R(32, 36), R(36, 37), NEG, op0=ALU.is_ge, op1=ALU.mult), [rt], [rt])
        dve(lambda e: e.tensor_tensor(R(37, 41), R(37, 41), R(32, 36), op=ALU.add), [rt], [rt])
        dve(lambda e: e.tensor_reduce(R(41, 42), R(37, 41), axis=AX.X, op=ALU.max), [rt], [rt])
        dve(lambda e: e.tensor_scalar(R(42, 46), R(32, 36), R(41, 42), None, op0=ALU.is_ge), [rt], [rt])
        dve(lambda e: e.tensor_scalar(R(46, 50), R(32, 36), R(36, 37), None, op0=ALU.subtract), [rt], [rt])
        act(lambda e: e.activation(R(46, 50), R(46, 50), AF.Exp), [rt], [rt])
        dve(lambda e: e.tensor_tensor(R(46, 50), R(46, 50), R(42, 46), op=ALU.mult), [rt], [rt])
        dve(lambda e: e.reduce_sum(R(50, 51), R(46, 50), axis=AX.X), [rt], [rt])
        dve(lambda e: e.reciprocal(R(51, 52), R(50, 51)), [rt], [rt])
        dve(lambda e: e.tensor_tensor(R(51, 52), R(51, 52), R(10, 11), op=ALU.mult), [rt], [rt])
        dve(lambda e: e.tensor_scalar(R(46, 50), R(46, 50), R(51, 52), None, op0=ALU.mult), [rt], [rt])
        dve(lambda e: e.tensor_tensor(comb[0:nt, :].rearrange("p (g j) -> p g j", j=4), R(1, 5).unsqueeze(2).to_broadcast([nt, 4, 4]),
                                      R(46, 50).unsqueeze(1).to_broadcast([nt, 4, 4]), op=ALU.mult), [rt], [comb])
        for ex in range(16):
            ea, eb, eT, ewd = ((pa, pb, actT, wdbuf), (rl[0], rl[1], aT, wdbuf2))[ex % 2]
            lin(hT_b, nt, 8, wgu_bf, ex * D, 0, 512, B[2 + ex % 2], B[2 + ex % 2][0:nt, :])
            gu = B[2 + ex % 2]
            load(ewd, ewd[:, :, :], wd_bf, wd_bf[ex * 256:(ex + 1) * 256, :].rearrange("(c p) n -> p c n", p=128))
            act(lambda e, gu=gu, ea=ea: e.activation(ea[0:nt, 0:256], gu[0:nt, 0:256], AF.Silu), [gu], [ea])
            dve(lambda e, gu=gu, ex=ex, ea=ea, eb=eb: e.scalar_tensor_tensor(eb[0:nt, 0:256], gu[0:nt, 256:512], comb[0:nt, ex:ex + 1], ea[0:nt, 0:256], op0=ALU.mult, op1=ALU.mult), [gu, comb, ea], [eb])
            transpose_tm(eb, 0, nt, 2, 128, eT, lambda c, eT=eT: eT[:, c, 0:nt], banks=((4,), (5,))[ex % 2])
            for half in range(2):
                for f in range(2):
                    mm(B[6 + half], B[6 + half][0:nt, :], eT, eT[:, f, 0:nt], ewd, ewd[:, f, half * 512:(half + 1) * 512],
                       ex == 0 and f == 0, ex == 15 and f == 1)
        for half in range(2):
            c0 = half * 512
            dve(lambda e, c0=c0, half=half: e.scalar_tensor_tensor(r_tm[0:nt, c0:c0 + 512], h1[0:nt, c0:c0 + 512], ALPHA, B[6 + half][0:nt, :], op0=ALU.mult, op1=ALU.add), [h1, B[6 + half]], [r_tm])
        layer_norm(r_tm, nt, 1, h1)
        load(y_out, y_out[yrow0:yrow0 + nt, :], h1, h1[0:nt, :], eng=PL)

    def prompt_mask(nq, nkeys):
        for hh in range(2):
            t = (pa, pb)[hh]
            c0 = nkeys - 1024 + hh * 512
            load(t, t[:, :], cmask_in, cmask_in[:, hh * 512:(hh + 1) * 512])
            dve(lambda e, t=t, c0=c0: e.tensor_tensor(Ibuf[0:nq, c0:c0 + 512], Ibuf[0:nq, c0:c0 + 512], t[0:nq, :], op=ALU.add), [Ibuf, t], [Ibuf])

    for j in range(NSLOT):
        n = 1 + 8 * j
        load_xT(xall, 16 + (n - 1) * 128, 128, x_tm, xT)
        mark('slot%d_start' % j)
        q_project(xT, 128, 0, n)
        mark('slot%d_qproj' % j)
        kcols = [(0, 16)] + [(16 + 128 * (t - 1), 128) for t in range(1, 8 * (j + 1) + 1)]
        nkeys = kcols[-1][0] + kcols[-1][1]
        indexer(128, nkeys, pik, iqT2, iw_sb, lambda h: iw_sb[0:128, h:h + 1], True)
        mark('slot%d_indexer' % j)
        threshold(128, nkeys, KP, prompt_mask)
        mark('slot%d_thr' % j)
        attn_core(128, kcols, pk, pv, 128, idrep, idrep[:, :].rearrange("p (r q) -> p r q", q=128), qT2,
                  lambda g: qT2[:, 2 * g:2 * g + 2, 0:128])

        def zsrc(nt, j=j):
            load(m_tm, m_tm[0:2, :], xhalo, xhalo[2 * j:2 * j + 2, :])
            transpose_tm(m_tm, 0, 2, 8, 128, xTh, lambda c: xTh[:, c, 0:2], banks=(0, 1))
            lin(xTh, 2, 8, win_bf, 0, O_CU, 512, B[0], B[0][0:2, :])
            lin(xTh, 2, 8, win_bf, 0, O_CC, 512, B[1], B[1][0:2, :])
            add_bias(pa, pa[0:2, :], B[0], B[0][0:2, :], 2, O_CU, 512)
            add_bias(pb, pb[0:2, :], B[1], B[1][0:2, :], 2, O_CC, 512)
            dve(lambda e: e.tensor_tensor(pa[0:2, :], pa[0:2, :], pb[0:2, :], op=ALU.mult), [pa, pb], [pa])
            load(z_scr, z_scr[0:2, :], pa, pa[0:2, :], eng=PL)
            load(z_scr, z_scr[2:130, :], z2, z2[0:128, :], eng=PL)
            load(z0, z0[0:128, :], z_scr, z_scr[0:128, :])
            load(z1, z1[0:128, :], z_scr, z_scr[1:129, :])
            if j == NSLOT - 1:
                load(cv_p, cv_p[:, :], z_scr, z_scr[128:130, :], eng=PL)
        mark('slot%d_attn' % j)
        tail(128, xT, 0, zsrc, y_p, j * 128)
        mark('slot%d_tail' % j)

    load(x_tm, x_tm[0:NTOKS, :], xs_in, xs_in[:, :])
    transpose_tm(x_tm, 0, NTOKS, 8, 128, xT, lambda c: xT[:, c, 0:NTOKS], banks=(0, 1))
    lin(xT, NTOKS, 8, win_bf, 0, O_K, 512, B[2], B[2][0:NTOKS, 0:512])
    lin(xT, NTOKS, 8, win_bf, 0, O_IK, 64, B[3], B[3][0:NTOKS, 0:64])
    add_bias(pa, pa[0:NTOKS, :], B[2], B[2][0:NTOKS, :], NTOKS, O_K, 512)
    add_bias(pb, pb[0:NTOKS, 0:64], B[3], B[3][0:NTOKS, 0:64], NTOKS, O_IK, 64)
    rope_tables("s")
    rope(pa, 0, 4, NTOKS, pc, 0, pd, pe_)
    rope(pb, 0, 1, NTOKS, pc, 256, pd, pe_)
    load(k_s, k_s[:, :], pc, pc[0:NTOKS, 0:256], eng=PL)
    load(v_s, v_s[:, :], pa, pa[0:NTOKS, 256:512], eng=PL)
    load(ik_s, ik_s[:, :], pc, pc[0:NTOKS, 256:320], eng=PL)
    transpose_tm(pc, 0, NTOKS, 4, 64, kst, lambda c: kst[:, c, 0:NTOKS], banks=(4, 5))
    transpose_tm(pc, 256, NTOKS, 1, 64, ikst, lambda c: ikst[:, 0:NTOKS], banks=(4, 5))
    dve(lambda e: e.tensor_copy(vst[0:NTOKS, :, 0:64], pa[0:NTOKS, 256:512].rearrange("p (h d) -> p h d", d=64)), [pa], [vst])
    for b in range(NSC):
        load(kTs_scr, kTs_scr[b * 64:(b + 1) * 64, :, NPG * 128:NPG * 128 + 4], kst, kst[:, :, 4 * b:4 * b + 4], eng=PL)
        load(ikTs_scr, ikTs_scr[b * 64:(b + 1) * 64, NPG * 128:NPG * 128 + 4], ikst, ikst[:, 4 * b:4 * b + 4], eng=PL)
        load(vs_scr, vs_scr[b * SPAD + NPG * 128:b * SPAD + NPG * 128 + 4, :], vst, vst[4 * b:4 * b + 4, :, :].rearrange("p h d -> p (h d)"), eng=PL)
    q_project(xT, NTOKS, 0, "s")
    qTs = mT
    iqTs = hT_b
    dve(lambda e: e.tensor_copy(qTs[0:64, :, 0:NTOKS], qT2[:, :, 0:NTOKS]), [qT2], [qTs])
    dve(lambda e: e.tensor_copy(iqTs[0:64, :, 0:NTOKS], iqT2[:, :, 0:NTOKS]), [iqT2], [iqTs])

    load(tri4, tri4[:, :], tri4_in, tri4_in[:, :])
    dve(lambda e: e.tensor_reduce(oh[0:NTOKS, :], ident[0:NTOKS, 0:NTOKS].rearrange("p (b q) -> p b q", q=4), axis=AX.X, op=ALU.add), [ident], [oh])
    dve(lambda e: e.tensor_tensor(iwm[0:NTOKS, :, :], iw_sb[0:NTOKS, :].unsqueeze(1).to_broadcast([NTOKS, NSC, 8]),
                                  oh[0:NTOKS, :].unsqueeze(2).to_broadcast([NTOKS, NSC, 8]), op=ALU.mult), [iw_sb, oh], [iwm])
    for r in range(4):
        dve(lambda e, r=r: e.tensor_copy(sel[0:NTOKS, :, r, :], ident[0:NTOKS, 0:NTOKS].rearrange("p (b q) -> p b q", q=4)), [ident], [sel])

    def sample_mask(nq, nkeys):
        dve(lambda e: e.tensor_tensor(Ibuf[0:nq, nkeys - 4:nkeys], Ibuf[0:nq, nkeys - 4:nkeys], tri4[0:nq, 0:4], op=ALU.add), [Ibuf, tri4], [Ibuf])

    SETW = 2064
    nextra = min(6, max(KPAD, PROJ) // SETW)
    xsets = []
    for i in range(nextra):
        o = i * SETW
        xsets.append((Tl(Bm.h[:, o:o + 1152].bitcast(F32), "xg%d" % i), None, None,
                      Tl(Bm.h[0:64, o + 1152:o + 1664].rearrange("p (g s) -> p g s", g=4), "xk%d" % i),
                      Tl(Bm.h[0:64, o + 1664:o + 1792], "xi%d" % i),
                      Tl(Bm.h[:, o + 1792:o + 2052].rearrange("p (h d) -> p h d", d=65), "xv%d" % i)))
    xflat = [t for st in xsets for t in st if t is not None]
    dve(lambda e: e.memset(Bm[:, 0:1], 0.0), [], [Bm] + xflat)
    for st in xsets:
        dve(lambda e, st=st: e.memset(st[5][:, :, :], 1.0), [], [st[5]])
    allsets = pgset + xsets
    NPB_ALL = len(allsets)
    pgc = 0
    for b in range(NSC):
        load(pt_i, pt_i[:, :], pt_in, pt_in[b:b + 1, :].partition_broadcast(128))
        dve(lambda e: e.tensor_copy(pt_f[:, :], pt_i[:, :]), [pt_i], [pt_f])
        dve(lambda e: e.tensor_scalar(pt_f[:, :], pt_f[:, :], 128.0, pidx[:, 0:1], op0=ALU.mult, op1=ALU.add), [pt_f, pidx], [pt_f])
        rws = rows_l[b % 2]
        dve(lambda e, rws=rws: e.tensor_copy(rws[:, :], pt_f[:, :]), [pt_f], [rws])
        for pg in range(NPG):
            st = allsets[pgc % NPB_ALL]
            pgc += 1
            gk, gv, gi, ks_, iks_, vs_ = st
            k.dma(PL, lambda e, dst=gk, pg=pg, rws=rws: e.indirect_dma_start(
                out=dst[:, :], out_offset=None, in_=ckv_in[:, :],
                in_offset=bass.IndirectOffsetOnAxis(ap=rws[:, pg:pg + 1], axis=0)), [ckv_in, rws], gk)
            bk2 = (4, 5) if pg % 2 == 0 else (2, 3)
            transpose_tm(gk, 0, 128, 4, 64, ks_, lambda c, ks_=ks_: ks_[:, c, 0:128], banks=(bk2[0],))
            transpose_tm(gk, 512, 128, 1, 64, iks_, lambda c, iks_=iks_: iks_[:, 0:128], banks=(bk2[1],))
            dve(lambda e, vs_=vs_, gk=gk: e.tensor_copy(vs_[:, :, 0:64], gk[:, 256:512].rearrange("p (h d) -> p h d", d=64)), [gk], [vs_])
            load(kTs_scr, kTs_scr[b * 64:(b + 1) * 64, :, pg * 128:(pg + 1) * 128], ks_, ks_[:, :, :])
            load(ikTs_scr, ikTs_scr[b * 64:(b + 1) * 64, pg * 128:(pg + 1) * 128], iks_, iks_[:, :])
            load(vs_scr, vs_scr[b * SPAD + pg * 128:b * SPAD + (pg + 1) * 128, :], vs_, vs_[:, :, :].rearrange("p h d -> p (h d)"))
        siks = (ikTs_scr, lambda c0, n, b=b: ikTs_scr[b * 64:(b + 1) * 64, c0:c0 + n])
        indexer(NTOKS, LS, siks, iqTs, iwm, lambda h, b=b: iwm[0:NTOKS, b, h:h + 1], b == 0)
    dve(lambda e: e.memset(Bm[:, 0:1], 0.0), [], [Bm] + xflat)
    mark('s_pages_idx')
    threshold(NTOKS, LS, KS, sample_mask)
    mark('s_thr')
    for b in range(NSC):
        kcols = [(128 * t, 128) for t in range(NPG)] + [(NPG * 128, 4)]
        sks = (kTs_scr, lambda c0, n, b=b: kTs_scr[b * 64:(b + 1) * 64, :, c0:c0 + n])
        svs = (vs_scr, lambda r0, n, b=b: vs_scr[b * SPAD + r0:b * SPAD + r0 + n, :])
        attn_core(4, kcols, sks, svs, NTOKS, sel, sel[0:NTOKS, b, :, :], qTs,
                  lambda g, b=b: qTs[0:64, 2 * g:2 * g + 2, 4 * b:4 * b + 4])
        load(at_scr, at_scr[4 * b:4 * b + 4, :], attn_o, attn_o[0:4, :], eng=PL)
    load(attn_o, attn_o[0:NTOKS, :], at_scr, at_scr[0:NTOKS, :])
    load(x_tm, x_tm[0:NTOKS, :], xs_in, xs_in[:, :])
    transpose_tm(x_tm, 0, NTOKS, 8, 128, xT, lambda c: xT[:, c, 0:NTOKS], banks=(0, 1))

    def zsrc_s(nt):
        for b in range(NSC):
            load(zs_scr, zs_scr[6 * b:6 * b + 2, :], sconv_in, sconv_in[2 * b:2 * b + 2, :], eng=PL)
            load(zs_scr, zs_scr[6 * b + 2:6 * b + 6, :], z2, z2[4 * b:4 * b + 4, :], eng=PL)
        for b in range(NSC):
            load(z0, z0[4 * b:4 * b + 4, :], zs_scr, zs_scr[6 * b:6 * b + 4, :])
            load(z1, z1[4 * b:4 * b + 4, :], zs_scr, zs_scr[6 * b + 1:6 * b + 5, :])
            load(cv_s, cv_s[2 * b:2 * b + 2, :], zs_scr, zs_scr[6 * b + 4:6 * b + 6, :], eng=PL)
    mark('s_attn')
    tail(NTOKS, xT, 0, zsrc_s, y_s, 0)
    mark('s_tail')
    import json as _json
    if cfg.get('MARKFILE'):
        _json.dump(MARK, open(cfg['MARKFILE'], 'w'))

    k.final_wait(PL, outs)
    with nc.Block() as block:
        @block.tensor
        def _(e):
            for f in PE.q:
                f(e)

        @block.scalar
        def _(e):
            for f in ACT.q:
                f(e)

        @block.vector
        def _(e):
            for f in DVE.q:
                f(e)

        @block.sync
        def _(e):
            for f in SP.q:
                f(e)

        @block.gpsimd
        def _(e):
            for f in PL.q:
                f(e)
    for cm in reversed(k.stack):
        cm.__exit__(None, None, None)
    print("instr counts", {e.name: len(e.q) for e in (PE, ACT, DVE, SP, PL)}, "sems", len(k.stack))
    return nc


def kernel(x_prompt, x_sample, cache_k, cache_v, cache_idx_k, state_conv, page_table, meta_tokens,
           w_in, b_in, w_conv, w_attn_up, w_conv_out, w_o, ln1_g, ln1_b,
           w_group, b_group, w_expert_router, b_expert_router, w_gate, w_up, w_down, ln2_g, ln2_b):
    f32 = np.float32
    A = lambda a: np.ascontiguousarray(np.asarray(a))
    x_prompt = A(x_prompt); x_sample = A(x_sample)
    SEQ = x_prompt.shape[1]
    NB = x_sample.shape[0]
    NPHYS = cache_k.shape[1]
    NPG = page_table.shape[1]
    PAST = NPG * 128
    NSC = NB // NCORES
    NQT = SEQ // 128
    NSLOT = NQT // NCORES
    NT = NQT + 1
    TP = SEQ + 16
    cfg = dict(SEQ=SEQ, NPHYS=NPHYS, NSC=NSC, NPG=NPG, KTOP_P=min(256, TP // 4), KTOP_S=min(256, (PAST + 4) // 4),
               ALPHA=float(2.0 ** 0.25), IDX_W_SCALE=float((8 ** -0.5) * (64 ** -0.5)))
    nc = build(cfg)
    xfull = np.concatenate([A(meta_tokens).astype(f32), x_prompt[0]], axis=0)
    tri = np.where(np.arange(128)[None, :] <= np.arange(128)[:, None], 0.0, NEG).astype(f32)
    ident = np.eye(128, dtype=f32)
    tri4 = np.where(np.arange(4)[None, :] <= (np.arange(128) % 4)[:, None], 0.0, NEG).astype(f32)
    invf = np.power(np.float32(10000.0), -np.arange(32, dtype=f32) * np.float32(2.0) / np.float32(64)).astype(f32)

    def cstable(pos):
        a = pos.astype(f32)[:, None] * invf[None, :]
        return np.concatenate([-np.cos(a), -np.sin(a)], axis=1).astype(f32)
    pidx = np.arange(128, dtype=f32)[:, None].copy()
    cstab_s = cstable(PAST + (np.arange(128) % 4))
    ckv = np.concatenate([A(cache_k)[0].reshape(NPHYS * 128, 256), A(cache_v)[0].reshape(NPHYS * 128, 256),
                          A(cache_idx_k)[0].reshape(NPHYS * 128, 64)], axis=1)
    common = dict(
        tri=tri, tri4=tri4, ident=ident, pidx=pidx, cstab_s=cstab_s, ckv=ckv,
        w_in=A(w_in)[0], b_in=A(b_in)[0][None, :], w_conv=A(w_conv)[0].reshape(1, 1536),
        w_a=A(w_attn_up)[0], w_b=A(w_conv_out)[0], w_o=A(w_o)[0],
        ln=np.stack([A(ln1_g)[0], A(ln1_b)[0], A(ln2_g)[0], A(ln2_b)[0]]).astype(f32),
        w_r=np.concatenate([A(w_group)[0], A(w_expert_router)[0]], axis=1),
        b_r=np.concatenate([A(b_group)[0], A(b_expert_router)[0]])[None, :],
        w_gate=A(w_gate)[0].reshape(16 * D, 256), w_up=A(w_up)[0].reshape(16 * D, 256),
        w_down=A(w_down)[0].reshape(16 * 256, D))
    in_maps = []
    for c in range(NCORES):
        perm = [c] + [r for r in range(8) if r != c]
        tiles = [8 * jb + perm[r] for jb in range(NSLOT) for r in range(8)]
        xall = np.concatenate([xfull[0:16]] + [xfull[16 + a * 128:16 + (a + 1) * 128] for a in tiles], axis=0)
        cstab = np.zeros((128, NT * 64), f32)
        cstab[:, 0:64] = cstable(np.arange(128))
        for n, a in enumerate(tiles):
            cstab[:, (n + 1) * 64:(n + 2) * 64] = cstable(16 + a * 128 + np.arange(128))
        xhalo = np.concatenate([xfull[16 + (8 * j + c) * 128 - 2:16 + (8 * j + c) * 128] for j in range(NSLOT)], axis=0)
        cmask = np.zeros((128, 1024), f32)
        cmask[:, 0:128] = tri
        for r in range(1, 8):
            if perm[r] > c:
                cmask[:, r * 128:(r + 1) * 128] = NEG
        m = dict(common)
        m.update(xall=xall, cstab=cstab, xhalo=xhalo, cmask=cmask,
                 xs=x_sample[c * NSC:(c + 1) * NSC].reshape(NSC * 4, D),
                 pt=A(page_table)[c * NSC:(c + 1) * NSC].astype(np.int32),
                 sconv=A(state_conv)[0, c * NSC:(c + 1) * NSC].reshape(NSC * 2, 512))
        in_maps.append(m)
    res = run_bass_kernel_spmd(nc, in_maps, core_ids=list(range(NCORES))).results
    y_prompt = np.zeros((1, SEQ, D), f32)
    k_prompt = np.zeros((1, 1, TP, 4, 64), f32); v_prompt = np.zeros((1, 1, TP, 4, 64), f32)
    ik_prompt = np.zeros((1, 1, TP, 64), f32)
    y_sample = np.zeros((NB, 4, D), f32)
    k_sample = np.zeros((1, NB, 4, 4, 64), f32); v_sample = np.zeros((1, NB, 4, 4, 64), f32)
    ik_sample = np.zeros((1, NB, 4, 64), f32)
    conv_sample = np.zeros((1, NB, 2, 512), f32)
    k_prompt[0, 0, 0:16] = res[0]["k_m"].reshape(16, 4, 64)
    v_prompt[0, 0, 0:16] = res[0]["v_m"].reshape(16, 4, 64)
    ik_prompt[0, 0, 0:16] = res[0]["ik_m"]
    for c in range(NCORES):
        r = res[c]
        for j in range(NSLOT):
            a = 8 * j + c
            y_prompt[0, a * 128:(a + 1) * 128] = r["y_p"][j * 128:(j + 1) * 128]
            k_prompt[0, 0, 16 + a * 128:16 + (a + 1) * 128] = r["k_p"][j * 128:(j + 1) * 128].reshape(128, 4, 64)
            v_prompt[0, 0, 16 + a * 128:16 + (a + 1) * 128] = r["v_p"][j * 128:(j + 1) * 128].reshape(128, 4, 64)
            ik_prompt[0, 0, 16 + a * 128:16 + (a + 1) * 128] = r["ik_p"][j * 128:(j + 1) * 128]
        y_sample[c * NSC:(c + 1) * NSC] = r["y_s"].reshape(NSC, 4, D)
        k_sample[0, c * NSC:(c + 1) * NSC] = r["k_s"].reshape(NSC, 4, 4, 64)
        v_sample[0, c * NSC:(c + 1) * NSC] = r["v_s"].reshape(NSC, 4, 4, 64)
        ik_sample[0, c * NSC:(c + 1) * NSC] = r["ik_s"].reshape(NSC, 4, 64)
        conv_sample[0, c * NSC:(c + 1) * NSC] = r["cv_s"].reshape(NSC, 2, 512)
    conv_prompt = res[NCORES - 1]["cv_p"].reshape(1, 1, 2, 512).astype(f32)
    return (y_prompt, y_sample, k_prompt, v_prompt, ik_prompt, conv_prompt,
            k_sample, v_sample, ik_sample, conv_sample)
```
